# Optimizing a Trainium2 kernel written in Bass

```python
import jax, jax.numpy as jnp
from jax import lax
import numpy as np

D_MODEL = 1024
BATCH = 2
SEQ = 8192
DEPTH = 1

CHUNK = 64
QBLK = 128
EPS = 1e-6

REC_HEADS = 4
REC_DK = 128
REC_DV = 128
REC_W = REC_HEADS * REC_DV
ATT_HEADS = 8
ATT_DH = 64
ATT_W = ATT_HEADS * ATT_DH
IDX_HEADS = 8
IDX_DIM = 64
TOPK_MAX = 256
NUM_BUCKETS = 32
MAX_DISTANCE = 128
N_GROUPS = 4
EXPERTS_PER_GROUP = 4
N_EXPERTS = N_GROUPS * EXPERTS_PER_GROUP
TOP_K_EXPERTS = 2
D_EXPERT = 512

MIX_W = REC_W + ATT_W
IN_SIZES = (REC_HEADS * REC_DK,
            REC_HEADS * REC_DK,
            REC_W,
            REC_W,
            ATT_W, ATT_W, ATT_W,
            IDX_HEADS * IDX_DIM,
            IDX_DIM,
            IDX_HEADS)
D_IN = int(sum(IN_SIZES))
IN_OFFSETS = tuple(int(o) for o in np.cumsum(IN_SIZES)[:-1])

kernel_name = 'hymba_hgrn2_dsa_hmoe_block'


def _rms(x, g):
    xf = x.astype(jnp.float32)
    y = xf * lax.rsqrt(jnp.mean(xf * xf, axis=-1, keepdims=True) + EPS)
    return (y * g.astype(jnp.float32)).astype(x.dtype)


def _layernorm(x, g, b):
    xf = x.astype(jnp.float32)
    mu = jnp.mean(xf, axis=-1, keepdims=True)
    var = jnp.mean(jnp.square(xf - mu), axis=-1, keepdims=True)
    y = (xf - mu) * lax.rsqrt(var + EPS)
    return (y * g.astype(jnp.float32) + b.astype(jnp.float32)).astype(x.dtype)


def _t5_bucket(rel):
    nb = NUM_BUCKETS // 2
    max_exact = nb // 2
    ret = jnp.where(rel > 0, nb, 0)
    n = jnp.abs(rel)
    nf = jnp.maximum(n, 1).astype(jnp.float32)
    large = max_exact + (jnp.log(nf / max_exact) / np.log(MAX_DISTANCE / max_exact)
                         * (nb - max_exact)).astype(jnp.int32)
    large = jnp.minimum(large, nb - 1)
    return ret + jnp.where(n < max_exact, n, large)


def _hgrn2(q_raw, f_raw, i_raw, g_raw, lb, out_g):
    B, S, _ = q_raw.shape
    nc = S // CHUNK
    f32 = jnp.float32
    q = jax.nn.silu(q_raw.astype(f32))
    f = lb + (1.0 - lb) * jax.nn.sigmoid(f_raw.astype(f32))
    k = 1.0 - f
    lf = jnp.log(f)
    v = i_raw.astype(f32)

    def to_chunks(a, d):
        return a.reshape(B, nc, CHUNK, REC_HEADS, d).transpose(1, 0, 3, 2, 4)

    tril = jnp.tril(jnp.ones((CHUNK, CHUNK), bool))[:, :, None]

    def step(state, inp):
        qc, kc, vc, lfc = inp
        b = jnp.cumsum(lfc, axis=2)
        diff = b[:, :, :, None, :] - b[:, :, None, :, :]
        decay = jnp.where(tril, jnp.exp(jnp.where(tril, diff, 0.0)), 0.0)
        attn = jnp.einsum('bhtk,bhsk,bhtsk->bhts', qc, kc, decay)
        o = (jnp.einsum('bhts,bhsv->bhtv', attn, vc)
             + jnp.einsum('bhtk,bhkv->bhtv', qc * jnp.exp(b), state))
        b_end = b[:, :, -1:, :]
        state = (jnp.exp(b_end[:, :, 0, :])[..., None] * state
                 + jnp.einsum('bhsk,bhsv->bhkv', kc * jnp.exp(b_end - b), vc))
        return state, o

    s0 = jnp.zeros((B, REC_HEADS, REC_DK, REC_DV), f32)
    _, o = lax.scan(step, s0, (to_chunks(q, REC_DK), to_chunks(k, REC_DK),
                               to_chunks(v, REC_DV), to_chunks(lf, REC_DK)))
    o = o.transpose(1, 0, 3, 2, 4).reshape(B, S, REC_HEADS, REC_DV)
    o = _rms(o, out_g.reshape(REC_HEADS, REC_DV))
    gate = jax.nn.silu(g_raw.astype(f32)).reshape(B, S, REC_HEADS, REC_DV)
    return (o * gate).reshape(B, S, REC_W).astype(q_raw.dtype)


def _dsa_attention(q, k, v, iq, ik, iw, rel_bias):
    B, S = q.shape[0], q.shape[1]
    nb = S // QBLK
    k_sel = min(TOPK_MAX, S // 4)
    key_chunk = jnp.arange(S) // CHUNK
    ikf = ik.astype(jnp.float32)

    def blocks(a):
        return a.reshape((B, nb, QBLK) + a.shape[2:]).swapaxes(0, 1)

    def one_block(args):
        bi, qb, iqb, iwb = args
        t = bi * QBLK + jnp.arange(QBLK)
        t_chunk = t // CHUNK
        sc = jnp.einsum('bqhd,bsd->bqhs', iqb.astype(jnp.float32), ikf) * IDX_DIM ** -0.5
        score = jnp.einsum('bqhs,bqh->bqs', jax.nn.relu(sc), iwb.astype(jnp.float32))
        adm = key_chunk[None, :] <= t_chunk[:, None]
        score = jnp.where(adm[None], score, -jnp.inf)
        _, idx = lax.top_k(score, k_sel)
        valid = (idx // CHUNK) <= t_chunk[None, :, None]
        kg = jax.vmap(lambda a, j: a[j])(k, idx)
        vg = jax.vmap(lambda a, j: a[j])(v, idx)
        logits = jnp.einsum('bqhd,bqkhd->bqhk', qb, kg).astype(jnp.float32) * ATT_DH ** -0.5
        bias = rel_bias[_t5_bucket(idx - t[None, :, None])]
        logits = logits + jnp.swapaxes(bias, 2, 3).astype(jnp.float32)
        logits = jnp.where(valid[:, :, None, :], logits, -jnp.inf)
        p = jax.nn.softmax(logits, axis=-1).astype(vg.dtype)
        return jnp.einsum('bqhk,bqkhd->bqhd', p, vg)

    out = lax.map(one_block, (jnp.arange(nb), blocks(q), blocks(iq), blocks(iw)))
    return out.swapaxes(0, 1).reshape(B, S, ATT_HEADS, ATT_DH)


def _hier_moe(h, w_rg, b_rg, w_re, b_re, w1, w3, w2):
    B, S, D = h.shape
    hf = h.reshape(B * S, D)
    g_logits = (hf @ w_rg + b_rg).astype(jnp.float32)
    p_group = jax.nn.softmax(g_logits, axis=-1)
    g_top = jnp.argmax(g_logits, axis=-1)
    gate_g = jnp.take_along_axis(p_group, g_top[:, None], axis=-1)
    e_logits = (hf @ w_re + b_re).astype(jnp.float32).reshape(-1, N_GROUPS, EXPERTS_PER_GROUP)
    e_in = jnp.take_along_axis(e_logits, g_top[:, None, None], axis=1)[:, 0]
    top_v, top_j = lax.top_k(e_in, TOP_K_EXPERTS)
    w_sel = jax.nn.softmax(top_v, axis=-1) * gate_g
    e_id = g_top[:, None] * EXPERTS_PER_GROUP + top_j
    combine = jnp.einsum('nk,nke->ne', w_sel,
                         jax.nn.one_hot(e_id, N_EXPERTS, dtype=jnp.float32)).astype(h.dtype)
    y = jnp.zeros_like(hf)
    for e in range(N_EXPERTS):
        he = jax.nn.silu(hf @ w1[e]) * (hf @ w3[e])
        y = y + combine[:, e:e + 1] * (he @ w2[e])
    return y.reshape(B, S, D)


def setup_inputs(seed: int = 0) -> dict:
    key = jax.random.key(seed)
    ks = jax.random.split(key, 24)
    f32 = jnp.float32

    def nrm(k, shape, s):
        return jax.random.normal(k, shape, f32) * s

    D = D_MODEL
    return {
        'x': nrm(ks[0], (BATCH, SEQ, D), 1.0),
        'c': nrm(ks[1], (BATCH, D), 1.0),
        'w_ada': nrm(ks[2], (DEPTH, D, 6 * D), 0.5 * D ** -0.5),
        'b_ada': nrm(ks[3], (DEPTH, 6 * D), 0.02),
        'norm1_g': 1.0 + nrm(ks[4], (DEPTH, D), 0.02),
        'norm2_g': 1.0 + nrm(ks[5], (DEPTH, D), 0.02),
        'w_in': nrm(ks[6], (DEPTH, D, D_IN), D ** -0.5),
        'lb_logits': nrm(ks[7], (DEPTH + 1, REC_HEADS * REC_DK), 0.5),
        'rec_out_g': 1.0 + nrm(ks[8], (DEPTH, REC_W), 0.02),
        'q_norm_g': 1.0 + nrm(ks[9], (DEPTH, ATT_DH), 0.02),
        'k_norm_g': 1.0 + nrm(ks[10], (DEPTH, ATT_DH), 0.02),
        'idx_k_norm_g': 1.0 + nrm(ks[11], (DEPTH, IDX_DIM), 0.02),
        'idx_k_norm_b': nrm(ks[12], (DEPTH, IDX_DIM), 0.02),
        'attn_out_g': 1.0 + nrm(ks[13], (DEPTH, ATT_W), 0.02),
        'rel_bias': nrm(ks[14], (NUM_BUCKETS, ATT_HEADS), 0.5),
        'w_out': nrm(ks[15], (DEPTH, MIX_W, D), MIX_W ** -0.5),
        'w_rg': nrm(ks[16], (DEPTH, D, N_GROUPS), D ** -0.5),
        'b_rg': nrm(ks[17], (DEPTH, N_GROUPS), 0.01),
        'w_re': nrm(ks[18], (DEPTH, D, N_EXPERTS), D ** -0.5),
        'b_re': nrm(ks[19], (DEPTH, N_EXPERTS), 0.01),
        'w1': nrm(ks[20], (DEPTH, N_EXPERTS, D, D_EXPERT), D ** -0.5),
        'w3': nrm(ks[21], (DEPTH, N_EXPERTS, D, D_EXPERT), D ** -0.5),
        'w2': nrm(ks[22], (DEPTH, N_EXPERTS, D_EXPERT, D), D_EXPERT ** -0.5),
    }


def reference(x, c, w_ada, b_ada, norm1_g, norm2_g, w_in, lb_logits, rec_out_g,
              q_norm_g, k_norm_g, idx_k_norm_g, idx_k_norm_b, attn_out_g, rel_bias,
              w_out, w_rg, b_rg, w_re, b_re, w1, w3, w2):
    B, S, D = x.shape
    lb_all = jnp.cumsum(jax.nn.softmax(lb_logits.astype(jnp.float32), axis=0), axis=0)
    for l in range(DEPTH):
        mod = jax.nn.silu(c) @ w_ada[l] + b_ada[l]
        sh1, sc1, g1, sh2, sc2, g2 = jnp.split(mod, 6, axis=-1)

        h = _rms(x, norm1_g[l]) * (1.0 + sc1[:, None, :]) + sh1[:, None, :]
        z = h @ w_in[l]
        rq, rf, ri, rg, aq, ak, av, iq, ik, iw = jnp.split(z, IN_OFFSETS, axis=-1)

        rec = _hgrn2(rq, rf, ri, rg, lb_all[l], rec_out_g[l])

        aq = _rms(aq.reshape(B, S, ATT_HEADS, ATT_DH), q_norm_g[l])
        ak = _rms(ak.reshape(B, S, ATT_HEADS, ATT_DH), k_norm_g[l])
        av = av.reshape(B, S, ATT_HEADS, ATT_DH)
        iq = iq.reshape(B, S, IDX_HEADS, IDX_DIM)
        ik = _layernorm(ik, idx_k_norm_g[l], idx_k_norm_b[l])
        iw = iw * IDX_HEADS ** -0.5
        att = _dsa_attention(aq, ak, av, iq, ik, iw, rel_bias)
        att = _rms(att, attn_out_g[l].reshape(ATT_HEADS, ATT_DH)).reshape(B, S, ATT_W)

        mix = jnp.concatenate([rec, att.astype(rec.dtype)], axis=-1) @ w_out[l]
        x = x + g1[:, None, :] * mix

        h2 = _rms(x, norm2_g[l]) * (1.0 + sc2[:, None, :]) + sh2[:, None, :]
        x = x + g2[:, None, :] * _hier_moe(h2, w_rg[l], b_rg[l], w_re[l], b_re[l],
                                           w1[l], w3[l], w2[l])
    return x
```

```python
import contextlib
import numpy as np
import ml_dtypes
import concourse.bass as bass
import concourse.mybir as mybir
from concourse.bass_utils import run_bass_kernel_spmd

F32 = mybir.dt.float32
BF16 = mybir.dt.bfloat16
ALU = mybir.AluOpType
AF = mybir.ActivationFunctionType
AX = mybir.AxisListType

EPOCH = 12000
D = 1024
S = 8192
NPT = 64
NTT = 32
TTW = 256
NB = 16
DIN = 4168
EPS = 1e-6
BIG = 30000.0
NBIS = 20
DEBUG = False


class Prog:
    ENG = ("pe", "act", "dve", "pool", "sp")

    def __init__(self, nc):
        self.nc = nc
        self.ops = {e: [] for e in self.ENG}
        self.cnt = {e: 0 for e in self.ENG}
        self.seen = {e: {} for e in self.ENG}
        self.last_w = {}
        self.readers = {}
        self.dma_cnt = {}
        self.fence = {}

    def barrier(self):
        f = {}
        for e in self.ENG:
            n = self.cnt[e]
            if n > 0:
                ep = (n - 1) // EPOCH
                f[("e", e, ep)] = n - ep * EPOCH
        for name, k in self.dma_cnt.items():
            f[("d", name)] = k
        self.fence = f

    def _eng_clock(self, eng):
        n = self.cnt[eng] + 1
        ep = (n - 1) // EPOCH
        return ("e", eng, ep), n - ep * EPOCH

    def _deps(self, eng, reads, writes):
        deps = {}

        def add(cv):
            if cv is None:
                return
            c, v = cv
            if c[0] == "d":
                v = self.dma_cnt[c[1]]
            if deps.get(c, 0) < v:
                deps[c] = v
        for r in reads:
            add(self.last_w.get(r))
        for w in writes:
            add(self.last_w.get(w))
            for c, v in self.readers.get(w, {}).items():
                add((c, v))
        for c, v in self.fence.items():
            if self.seen[eng].get(c, 0) < v:
                add((c, v))
        out = []
        for c, v in deps.items():
            if c[0] == "e" and c[1] == eng and eng == "pe":
                continue
            if self.seen[eng].get(c, 0) >= v:
                continue
            self.seen[eng][c] = v
            out.append((c, v))
        return out

    def _mark(self, clock, val, reads, writes):
        for r in reads:
            d = self.readers.setdefault(r, {})
            if d.get(clock, 0) < val:
                d[clock] = val
        for w in writes:
            self.last_w[w] = (clock, val)
            self.readers[w] = {}

    def op(self, eng, fn, reads=(), writes=()):
        waits = self._deps(eng, reads, writes)
        clock, val = self._eng_clock(eng)
        self.cnt[eng] += 1
        self.ops[eng].append(("op", waits, fn, clock, val))
        self._mark(clock, val, reads, writes)

    def dma(self, q, semname, out, in_, reads=(), writes=(), **kw):
        waits = self._deps(q, reads, writes)
        k = self.dma_cnt.get(semname, 0) + 16
        self.dma_cnt[semname] = k
        clock = ("d", semname)
        self.ops[q].append(("dma", waits, (out, in_, kw), clock, k))
        self._mark(clock, k, reads, writes)

    def final_wait(self, eng, resources):
        waits = self._deps(eng, resources, ())
        self.ops[eng].append(("wait", waits, None, None, None))

    def emit(self):
        nc = self.nc
        clocks = set()
        for e in self.ENG:
            for o in self.ops[e]:
                for c, v in o[1]:
                    clocks.add(c)
                if o[3] is not None:
                    clocks.add(o[3])
        clocks = sorted(clocks, key=str)
        with contextlib.ExitStack() as st:
            sems = {}
            for i, c in enumerate(clocks):
                sems[c] = st.enter_context(nc.semaphore("s%d" % i))
            block = st.enter_context(nc.Block())

            def run(engname, eng):
                for kind, waits, payload, clock, val in self.ops[engname]:
                    for c, v in waits:
                        eng.wait_ge(sems[c], v)
                    if kind == "op":
                        payload(eng).then_inc(sems[clock], 1)
                    elif kind == "dma":
                        out, in_, kw = payload
                        eng.dma_start(out=out, in_=in_, **kw).then_inc(sems[clock], 16)

            @block.tensor
            def _(e):
                run("pe", e)

            @block.scalar
            def _(e):
                run("act", e)

            @block.vector
            def _(e):
                run("dve", e)

            @block.gpsimd
            def _(e):
                run("pool", e)

            @block.sync
            def _(e):
                run("sp", e)


def _t5_onehot():
    import jax
    import jax.numpy as jnp
    with jax.default_device(jax.devices("cpu")[0]):
        rel = jnp.arange(384, dtype=jnp.int32) - 255
        nb = 16
        max_exact = 8
        ret = jnp.where(rel > 0, nb, 0)
        n = jnp.abs(rel)
        nf = jnp.maximum(n, 1).astype(jnp.float32)
        large = max_exact + (jnp.log(nf / max_exact) / np.log(128 / max_exact)
                             * (nb - max_exact)).astype(jnp.int32)
        large = jnp.minimum(large, nb - 1)
        bucket = np.asarray(ret + jnp.where(n < max_exact, n, large))
    oh = np.zeros((32, 384), np.float32)
    oh[bucket, np.arange(384)] = 1.0
    oh[15, :] -= 1.0
    return oh


def _consts():
    c = {}
    idx = np.arange(128)
    c["ident_b"] = np.eye(128, dtype=np.float32).astype(ml_dtypes.bfloat16)
    c["ident_f"] = np.eye(128, dtype=np.float32)
    c["i30k_b"] = (np.eye(128, dtype=np.float32) * BIG).astype(ml_dtypes.bfloat16)
    c["i30k4_b"] = np.tile(np.eye(128, dtype=np.float32) * BIG, (1, 4)).astype(ml_dtypes.bfloat16)
    c["jmat_b"] = np.eye(128, dtype=np.float32)[::-1].copy().astype(ml_dtypes.bfloat16)
    ch = idx // 64
    mid = ch * 64 + 31
    same = ch[:, None] == ch[None, :]
    tri = (idx[:, None] <= idx[None, :]) & same
    tm = (idx[:, None] <= mid[None, :]) & same
    c["d1"] = (tri.astype(np.float32) - tm.astype(np.float32))
    cm = np.zeros((128, 6), np.float32)
    for cc in range(2):
        inc = ch == cc
        cm[:, 3 * cc + 0] = inc & (idx <= cc * 64 + 31)
        cm[:, 3 * cc + 1] = inc
        cm[:, 3 * cc + 2] = inc & (idx > cc * 64 + 31)
    c["cm"] = cm
    c["hmask"] = (same & (idx[None, :] >= idx[:, None])).astype(np.float32)
    c["oh"] = _t5_onehot()
    u64 = np.zeros((1, 128), np.float32)
    u64[0, :64] = 1.0
    c["u64"] = u64.astype(ml_dtypes.bfloat16)
    pen = np.zeros((1, 512), np.float32)
    pen[0, 448:] = -BIG
    c["penrow"] = pen.astype(ml_dtypes.bfloat16)
    sel = np.zeros((16, 16, 128), np.float32)
    for e in range(16):
        sel[e, e, :] = 1.0
    c["sel"] = sel.transpose(1, 0, 2).reshape(16, 16 * 128).astype(ml_dtypes.bfloat16)
    return c


CONST_SPECS = [
    ("ident_b", [128, 128], BF16), ("ident_f", [128, 128], F32), ("i30k_b", [128, 128], BF16), ("i30k4_b", [128, 512], BF16),
    ("jmat_b", [128, 128], BF16), ("d1", [128, 128], F32), ("cm", [128, 6], F32),
    ("hmask", [128, 128], F32), ("oh", [32, 384], F32), ("u64", [1, 128], BF16),
    ("penrow", [1, 512], BF16), ("sel", [16, 2048], BF16),
]

IN_SPECS = [
    ("xT", [D, S], F32), ("x_own", [NB * 128, D], F32), ("valid", [128, NPT], F32), ("dpen", [1, 512], BF16),
    ("c_col", [128, 8], F32), ("w_ada", [D, 6 * D], F32), ("b_ada", [1, 6 * D], F32),
    ("n1g_col", [128, 8], F32), ("n2g", [1, D], F32), ("w_in", [D, DIN], F32), ("lb_logits", [2, 512], F32),
    ("rec_out_g", [1, 512], F32), ("q_norm_g", [1, 64], F32), ("k_norm_g", [1, 64], F32),
    ("ikg", [1, 64], F32), ("ikb", [1, 64], F32), ("attn_out_g", [1, 512], F32), ("rel_bias", [32, 8], F32),
    ("w_out", [D, D], F32), ("w_r", [D, 20], F32), ("b_r", [1, 20], F32),
    ("w1", [16 * D, 512], F32), ("w3", [16 * D, 512], F32), ("w2", [16 * 512, D], F32),
]


def build_program():
    nc = bass.Bass("TRN2", target_bir_lowering=False)
    P = Prog(nc)
    T = {}
    for name, shape, dt in IN_SPECS + CONST_SPECS:
        T[name] = nc.dram_tensor(name, shape, dt, kind="ExternalInput").ap()
    out_d = nc.dram_tensor("out", [NB * 128, D], F32, kind="ExternalOutput").ap()
    dbg = {}

    mod_scr = nc.dram_tensor("mod_scr", [1, 6 * D], F32, kind="Internal")
    kT_scr = nc.dram_tensor("kT_scr", [4 * 128, S], BF16, kind="Internal")
    v_scr = nc.dram_tensor("v_scr", [S, 8 * 65], BF16, kind="Internal")
    qT_scr = nc.dram_tensor("qT_scr", [NB * 128, 4 * 128], BF16, kind="Internal")
    iqT_scr = nc.dram_tensor("iqT_scr", [NB * 128, 4 * 128], BF16, kind="Internal")
    fv_scr = nc.dram_tensor("fv_scr", [8, 384], F32, kind="Internal")
    rec_scr = nc.dram_tensor("rec_scr", [NB * 128, 512], BF16, kind="Internal")

    ARENA = 212480
    arena = nc.alloc_sbuf_tensor("arena", [128, ARENA // 4], F32)

    def view(off, shape, dt, parts=128):
        esz = 2 if dt == BF16 else 4
        n = int(np.prod(shape))
        assert off % 4 == 0 and off + n * esz <= ARENA, (off, shape)
        a = arena[0:parts, off // 4: off // 4 + (n * esz + 3) // 4]
        if dt == BF16:
            a = a.bitcast(BF16)[:, 0:n]
        if len(shape) == 2:
            a = a.rearrange("p (a b) -> p a b", a=shape[0])
        elif len(shape) == 3:
            a = a.rearrange("p (a b c) -> p a b c", a=shape[0], b=shape[1])
        return a

    class Alloc:
        def __init__(self, base, limit):
            self.o = base
            self.limit = limit

        def __call__(self, shape, dt, parts=128):
            esz = 2 if dt == BF16 else 4
            n = int(np.prod(shape)) * esz
            n = (n + 31) // 32 * 32
            v = view(self.o, shape, dt, parts)
            self.o += n
            assert self.o <= self.limit, (self.o, self.limit)
            return v

    psall = nc.alloc_psum_tensor("psall", [128, 4096], F32)
    bank_state = {"i": 0, "set": list(range(8))}

    def bank(n=1):
        s = bank_state["set"]
        while True:
            i = bank_state["i"] % len(s)
            ids = s[i:i + n]
            bank_state["i"] += 1
            if len(ids) == n and all(ids[k] + 1 == ids[k + 1] for k in range(n - 1)):
                bank_state["i"] += n - 1
                return ids

    def psv(b, lo=0, hi=512):
        return psall[:, b * 512 + lo: b * 512 + hi]

    def psb(b, lo=0, hi=1024):
        return psall[:, b * 512: (b + 1) * 512].bitcast(BF16)[:, lo:hi]

    def R(b):
        return "ps%d" % b

    def mm(out, lhsT, rhs, start, stop, reads, writes, skip=False):
        if skip:
            P.op("pe", lambda e: e.matmul(out, lhsT, rhs, start=start, stop=stop, skip_group_check=True), reads=reads, writes=writes)
        else:
            P.op("pe", lambda e: e.matmul(out, lhsT, rhs, start=start, stop=stop), reads=reads, writes=writes)

    def tr(out, in_, ident, reads, writes):
        P.op("pe", lambda e: e.transpose(out, in_, ident), reads=reads, writes=writes)

    def act(out, in_, func, reads, writes, **kw):
        P.op("act", lambda e: e.activation(out=out, in_=in_, func=func, **kw), reads=reads, writes=writes)

    def tt(eng, out, in0, in1, op, reads, writes):
        P.op(eng, lambda e: e.tensor_tensor(out, in0, in1, op), reads=reads, writes=writes)

    def ts(eng, out, in0, s1, s2, op0, op1, reads, writes, accum_out=None):
        if accum_out is None:
            P.op(eng, lambda e: e.tensor_scalar(out, in0, s1, s2, op0, op1), reads=reads, writes=writes)
        else:
            P.op(eng, lambda e: e.tensor_scalar(out, in0, s1, s2, op0, op1, accum_out=accum_out), reads=reads, writes=writes)

    def cp(eng, out, in_, reads, writes):
        P.op(eng, lambda e: e.tensor_copy(out, in_), reads=reads, writes=writes)

    def ms(eng, out, val, writes):
        P.op(eng, lambda e: e.memset(out, val), writes=writes)

    def rsqrt_small(tag, out, in_, scale, reads):
        act(out, in_, AF.Sqrt, reads=list(reads) + ["eps"], writes=[tag], scale=scale, bias=eps_col[0:out.shape[0], :])
        P.op("dve", lambda e: e.reciprocal(out, out), reads=[tag], writes=[tag])

    pa = Alloc(0, 48 * 1024)
    ident_b = pa([128], BF16)
    i30k_b = pa([128], BF16)
    jmat_b = pa([128], BF16)
    ident_f = pa([128], F32)
    d1 = pa([128], F32)
    cm = pa([6], F32)
    hmask = pa([128], F32)
    ones_cb = pa([2], BF16)
    ones_row = pa([128], BF16, parts=1)
    u64 = pa([128], BF16, parts=1)
    penrow = pa([512], BF16, parts=1)
    dpen = pa([512], BF16, parts=1)
    eps_col = pa([1], F32)
    valid = pa([NPT], F32)
    oml_bc = pa([512], F32)
    recg_bc = pa([512], F32)
    attg_bc = pa([512], F32)
    qg_bc = pa([64], F32)
    kg_bc = pa([64], F32)
    ikg_bc = pa([64], F32)
    ikb_bc = pa([64], F32)
    iw_all = pa([NB, 8], F32)
    att_all = pa([NB, 512], BF16)
    small = pa([64], F32)
    sh1c = pa([8], F32)
    a1c = pa([8], F32)
    n1gc = pa([8], F32)
    PERS_END = pa.o

    ld = lambda name, dst, q="sp", **kw: P.dma(q, "c_" + name, dst, T[name], writes=[name], **kw)
    ld("ident_b", ident_b); ld("i30k_b", i30k_b); ld("jmat_b", jmat_b); ld("ident_f", ident_f)
    ld("d1", d1); ld("cm", cm); ld("hmask", hmask); ld("u64", u64); ld("penrow", penrow); ld("dpen", dpen)
    ld("valid", valid)
    ms("pool", ones_cb, 1.0, ["ones_cb"])
    ms("pool", ones_row, 1.0, ["ones_row"])
    ms("pool", eps_col, EPS, ["eps"])
    bcl = lambda name, dst, n: P.dma("sp", "c_" + name, dst, T[name].to_broadcast([128, n]), writes=[name])
    bcl("rec_out_g", recg_bc, 512); bcl("attn_out_g", attg_bc, 512)
    bcl("q_norm_g", qg_bc, 64); bcl("k_norm_g", kg_bc, 64); bcl("ikg", ikg_bc, 64); bcl("ikb", ikb_bc, 64)
    ts("dve", qg_bc, qg_bc, 0.125, None, ALU.mult, ALU.bypass, ["q_norm_g"], ["q_norm_g"])
    A0 = PERS_END
    al = Alloc(A0, ARENA)
    wA = al([8, DIN], BF16)
    crow = al([DIN + 24], BF16, parts=1)
    ikT = al([S], BF16)
    Sst = al([4, 128], F32)
    A_WORK = al.o
    st_al = Alloc(A_WORK, ARENA)
    wst = [st_al([DIN], F32), st_al([DIN], F32)]
    lbt = st_al([2, 512], F32)
    sc_col = st_al([8], F32)
    ccol = st_al([8], F32)
    wada = [st_al([8, 512], F32), st_al([8, 512], F32)]
    brow = st_al([512], F32, parts=1)
    mrow = [st_al([512], F32, parts=1), st_al([512], F32, parts=1)]
    sh1b = st_al([8], BF16)

    for k in range(8):
        wsb = wst[k % 2]
        P.dma("act", "wst%d" % (k % 2), wsb, T["w_in"][k * 128:(k + 1) * 128, :], writes=["wst%d" % (k % 2)])
        cp("dve" if k % 2 == 0 else "pool", wA[:, k, :], wsb, ["wst%d" % (k % 2)], ["wA"])

    P.dma("sp", "c_lb", lbt[:, 0, :], T["lb_logits"][0:1, :].to_broadcast([128, 512]), writes=["lb0"])
    P.dma("sp", "c_lb", lbt[:, 1, :], T["lb_logits"][1:2, :].to_broadcast([128, 512]), writes=["lb1"])
    tt("dve", lbt[:, 0, :], lbt[:, 1, :], lbt[:, 0, :], ALU.subtract, ["lb0", "lb1"], ["lb0"])
    act(oml_bc, lbt[:, 0, :], AF.Sigmoid, ["lb0"], ["oml"])

    ld("c_col", ccol)
    act(sc_col, ccol, AF.Silu, ["c_col"], ["sc_col"])
    wada_v = T["w_ada"].rearrange("(k p) c -> p k c", p=128)
    for g in range(12):
        wb = wada[g % 2]
        P.dma("sp", "wada%d" % (g % 2), wb, wada_v[:, :, g * 512:(g + 1) * 512], writes=["wada%d" % (g % 2)])
        P.dma("sp", "brow", brow, T["b_ada"][:, g * 512:(g + 1) * 512], writes=["brow"])
        b = bank()[0]
        for k in range(8):
            mm(psv(b)[0:1, :], sc_col[:, k:k + 1], wb[:, k, :], k == 0, k == 7,
               ["sc_col", "wada%d" % (g % 2)], [R(b)])
        mr = mrow[g % 2]
        tt("dve", mr, psv(b)[0:1, :], brow, ALU.add, [R(b), "brow"], ["mrow%d" % (g % 2)])
        P.dma("sp", "modw", mod_scr.ap()[:, g * 512:(g + 1) * 512], mr, reads=["mrow%d" % (g % 2)], writes=["mod_scr"])
    modv = mod_scr.ap()
    P.dma("sp", "c_sh1", sh1c, modv[0, 0:1024].rearrange("(k p) -> p k", p=128), reads=["mod_scr"], writes=["sh1c"],
          allow_slow_non_contiguous=True)
    P.dma("sp", "c_sc1", a1c, modv[0, 1024:2048].rearrange("(k p) -> p k", p=128), reads=["mod_scr"], writes=["a1c"],
          allow_slow_non_contiguous=True)
    ld("n1g_col", n1gc)
    ts("dve", a1c, a1c, 1.0, None, ALU.add, ALU.bypass, ["a1c"], ["a1c"])
    tt("dve", a1c, a1c, n1gc, ALU.mult, ["a1c", "n1g_col"], ["a1c"])

    cp("dve", sh1b, sh1c, ["sh1c"], ["sh1b"])
    for g in range(9):
        col0 = g * 512
        w_ = min(512, DIN - col0)
        b = bank()[0]
        for k in range(8):
            mm(psv(b, 0, w_)[0:1, :], sh1b[:, k:k + 1], wA[:, k, col0:col0 + w_], k == 0, k == 7, ["sh1b", "wA"], [R(b)])
        cp("dve", crow[:, col0:col0 + w_], psv(b, 0, w_)[0:1, :], [R(b)], ["crow"])
    ms("pool", Sst, 0.0, ["S"])

    rb_s = st_al([8], F32)
    oh_s = st_al([384], F32)
    fv_s = st_al([384], F32)
    P.dma("sp", "c_rb", rb_s[0:32, :], T["rel_bias"], writes=["rb"])
    P.dma("sp", "c_oh", oh_s[0:32, :], T["oh"], writes=["oh"])
    mm(psv(1)[0:8, 0:384], rb_s[0:32, :], oh_s[0:32, :], True, True, ["rb", "oh"], [R(1)])
    cp("dve", fv_s[0:8, :], psv(1)[0:8, 0:384], [R(1)], ["fv_s"])
    P.dma("sp", "fvw", fv_scr.ap(), fv_s[0:8, :], reads=["fv_s"], writes=["fv_scr"])

    wk = Alloc(A_WORK, ARENA)
    xt = [wk([8, TTW], F32), wk([8, TTW], F32)]
    xb = wk([8, TTW], BF16)
    xsq = wk([8, TTW], BF16)
    PB = lambda shape, dt, **kw: [wk(shape, dt, **kw), wk(shape, dt, **kw)]
    f_a = PB([512], F32); f_b = PB([512], F32); f_c = PB([512], F32); f_d = PB([512], F32); f_f = PB([512], F32)
    f_g = PB([512], BF16)
    ktil = PB([512], BF16); vbf = PB([512], BF16); knb = PB([512], BF16)
    vext = PB([8, 65], BF16)
    kTt = PB([4, 128], BF16)
    ikn2 = PB([128], BF16)
    ikr_ = PB([64], F32)
    ecols = PB([24], F32)
    colt = PB([32], F32)
    rmsrow = PB([128], BF16, parts=1)
    f_e = wk([512], F32)
    qtil = wk([512], BF16)
    tmp4 = wk([4, 128], F32)
    Sm = [wk([4, 128], BF16), wk([4, 128], BF16)]
    kq_T = wk([8, 128], BF16)
    A_T = wk([4, 128], BF16)
    recraw = wk([512], F32)
    qTt = wk([4, 128], BF16)
    recb = wk([512], BF16)
    iqTt = wk([4, 128], BF16)
    A_END = wk.o

    P.barrier()
    ms("pool", vext[0], 1.0, ["vext0"])
    ms("pool", vext[1], 1.0, ["vext1"])

    xT_v = T["xT"].rearrange("(k p) t -> p k t", p=128)

    def proj(q, col0, width, s_):
        b = bank()[0]
        for k in range(8):
            mm(psv(b, 0, width), xb[:, k, q * 128:(q + 1) * 128], wA[:, k, col0:col0 + width], k == 0, False,
               ["xb", "wA"], [R(b)])
        mm(psv(b, 0, width), rmsrow[s_][0:1, :], crow[0:1, col0:col0 + width], False, True, ["rmsrow%d" % s_, "crow"], [R(b)])
        return b

    def headnorm(src, nh, hd, gbc, dst, tagp, gtag, s_, dtag=None, eng2="pool"):
        sq = f_g[s_]
        sqn = "f_g%d" % s_
        cn = "colt_ss%d" % s_
        act(sq[:, 0:nh * hd], src, AF.Square, [tagp], [sqn])
        ss = colt[s_][:, 0:nh]
        P.op("dve", lambda e: e.tensor_reduce(ss, sq[:, 0:nh * hd].rearrange("p (h d) -> p h d", h=nh), AX.X, ALU.add),
             reads=[sqn], writes=[cn])
        rsqrt_small(cn, ss, ss, 1.0 / hd, [cn])
        s3 = src.rearrange("p (h d) -> p h d", h=nh)
        tt("dve", s3, s3, ss.unsqueeze(2).to_broadcast([128, nh, hd]), ALU.mult, [tagp, cn], [tagp])
        g3 = gbc.unsqueeze(1).to_broadcast([128, nh, hd]) if gbc.shape[1] == hd else gbc.rearrange("p (h d) -> p h d", h=nh)
        tt(eng2, dst.rearrange("p (h d) -> p h d", h=nh), s3, g3, ALU.mult, [tagp, gtag], [dtag or tagp])

    f_q = wk([512], F32); f_gate = wk([512], F32); f_aq = wk([512], F32)
    iqb = wk([512], BF16)

    def xload(t_):
        P.dma("sp", "xt%d" % (t_ % 2), xt[t_ % 2], xT_v[:, :, t_ * TTW:(t_ + 1) * TTW], writes=["xt%d" % (t_ % 2)])

    def xcast(tti):
        xtb = xt[tti % 2]
        xr = "xt%d" % (tti % 2)
        tt("dve", xb, xtb, a1c.unsqueeze(2).to_broadcast([128, 8, TTW]), ALU.mult, [xr, "a1c"], ["xb"])
        act(xsq, xtb, AF.Square, [xr], ["xsq"])
        if tti + 1 < NTT:
            xload(tti + 1)

    def early(p):
        q = p % (TTW // 128)
        own = (p % 4 == 3)
        i = p // 4
        s_ = p % 2
        N = lambda base: base + str(s_)
        b = bank()[0]
        for k in range(8):
            mm(psv(b, 0, 1), xsq[:, k, q * 128:(q + 1) * 128], ones_cb[:, 0:1], k == 0, k == 7, ["xsq", "ones_cb"], [R(b)])
        for k in range(8):
            mm(psv(b, 128, 256)[0:1, :], ones_cb[:, 0:1], xsq[:, k, q * 128:(q + 1) * 128], k == 0, k == 7, ["xsq", "ones_cb"], [R(b)])
        act(rmsrow[s_], psv(b, 128, 256)[0:1, :], AF.Sqrt, [R(b), "eps"], [N("rmsrow")], scale=1.0 / D, bias=eps_col[0:1, :])
        ct = colt[s_]
        rstd = ct[:, 16:17]; nrstd = ct[:, 17:18]; rstd8 = ct[:, 18:19]
        rs = N("rstd")
        act(rstd, psv(b, 0, 1), AF.Sqrt, [R(b), "eps"], [rs], scale=1.0 / D, bias=eps_col)
        P.op("dve", lambda e, o=rstd: e.reciprocal(o, o), reads=[rs], writes=[rs])
        tt("dve", rstd, rstd, valid[:, p:p + 1], ALU.mult, [rs, "valid"], [rs])
        ts("dve", nrstd, rstd, -1.0, None, ALU.mult, ALU.bypass, [rs], [N("nrstd")])
        b_rf = proj(q, 512, 512, s_)
        act(f_a[s_], psv(b_rf), AF.Sigmoid, [R(b_rf), N("nrstd")], [N("f_a")], scale=nrstd)
        tt("dve", f_b[s_], f_a[s_], oml_bc, ALU.mult, [N("f_a"), "oml"], [N("f_b")])
        act(f_c[s_], f_b[s_], AF.Ln, [N("f_b")], [N("f_c")], scale=-1.0, bias=1.0)
        b_ri = proj(q, 1024, 512, s_)
        ts("dve", vbf[s_], psv(b_ri), rstd, None, ALU.mult, ALU.bypass, [R(b_ri), rs], [N("vbf")])
        b_ak = proj(q, 2560, 512, s_)
        ts("dve", f_f[s_], psv(b_ak), rstd, None, ALU.mult, ALU.bypass, [R(b_ak), rs], [N("f_f")])
        b_av = proj(q, 3072, 512, s_)
        ve = vext[s_]
        ts("dve", ve[:, :, 0:64], psv(b_av).rearrange("p (h d) -> p h d", h=8), rstd, None, ALU.mult, ALU.bypass,
           [R(b_av), rs], [N("vext")])
        P.dma("sp", N("vw"), v_scr.ap()[p * 128:(p + 1) * 128, :].rearrange("s (h d) -> s h d", h=8), ve,
              reads=[N("vext")], writes=["v_scr"])
        b_ik = proj(q, 4096, 64, s_)
        ts("dve", ikr_[s_], psv(b_ik, 0, 64), rstd, None, ALU.mult, ALU.bypass, [R(b_ik), rs], [N("ikr")])
        if own:
            b_rq = proj(q, 0, 512, s_)
            act(f_q, psv(b_rq), AF.Silu, [R(b_rq), rs], ["f_q"], scale=rstd)
            b_rg = proj(q, 1536, 512, s_)
            act(f_gate, psv(b_rg), AF.Silu, [R(b_rg), rs], ["f_gate"], scale=rstd)
            b_aq = proj(q, 2048, 512, s_)
            ts("dve", f_aq, psv(b_aq), rstd, None, ALU.mult, ALU.bypass, [R(b_aq), rs], ["f_aq"])
            b_iq = proj(q, 3584, 512, s_)
            ts("dve", rstd8, rstd, 0.125, None, ALU.mult, ALU.bypass, [rs], [N("rstd8")])
            ts("dve", iqb, psv(b_iq), rstd8, None, ALU.mult, ALU.bypass, [R(b_iq), N("rstd8")], ["iqb"])
            b_iw = proj(q, 4160, 8, s_)
            ts("dve", rstd8, rstd, float(8 ** -0.5), None, ALU.mult, ALU.bypass, [rs, N("rstd8")], [N("rstd8")])
            ts("dve", iw_all[:, i, :], psv(b_iw, 0, 8), rstd8, None, ALU.mult, ALU.bypass, [R(b_iw), N("rstd8")], ["iw%d" % i])

    def late(p):
        own = (p % 4 == 3)
        i = p // 4
        s_ = p % 2
        N = lambda base: base + str(s_)
        ct = colt[s_]
        ikr = ikr_[s_]
        b_b = bank()[0]
        mm(psv(b_b), d1, f_c[s_], True, True, ["d1", N("f_c")], [R(b_b)])
        b_c = bank()[0]
        for h in range(4):
            mm(psv(b_c, h * 6, h * 6 + 6), f_c[s_][:, h * 128:(h + 1) * 128], cm, True, True, [N("f_c"), "cm"], [R(b_c)])
        act(f_d[s_], psv(b_b), AF.Exp, [R(b_b)], [N("f_d")], scale=-1.0)
        tt("dve", ktil[s_], f_b[s_], f_d[s_], ALU.mult, [N("f_b"), N("f_d")], [N("ktil")])
        act(ecols[s_], psv(b_c, 0, 24), AF.Exp, [R(b_c)], [N("ecols")])
        if own:
            act(f_e, psv(b_b), AF.Exp, [R(b_b)], ["f_e"])
        headnorm(f_f[s_], 8, 64, kg_bc, knb[s_], N("f_f"), "k_norm_g", s_, dtag=N("knb"), eng2="dve")
        st6 = ct[:, 20:26]; mv = ct[:, 26:28]; irs = ct[:, 28:29]
        P.op("dve", lambda e, o=st6, i_=ikr: e.bn_stats(o, i_), reads=[N("ikr")], writes=[N("st6")])
        P.op("dve", lambda e, o=mv, i_=st6: e.bn_aggr(o, i_), reads=[N("st6")], writes=[N("mv")])
        rsqrt_small(N("ikrs"), irs, mv[:, 1:2], 1.0, [N("mv")])
        ts("dve", ikr, ikr, mv[:, 0:1], irs, ALU.subtract, ALU.mult, [N("ikr"), N("mv"), N("ikrs")], [N("ikr")])
        tt("dve", ikr, ikr, ikg_bc, ALU.mult, [N("ikr"), "ikg"], [N("ikr")])
        tt("dve", ikn2[s_][:, 0:64], ikr, ikb_bc, ALU.add, [N("ikr"), "ikb"], [N("ikn2")])
        cp("pool", ikn2[s_][:, 64:128], ikn2[s_][:, 0:64], [N("ikn2")], [N("ikn2")])

    def late2(p):
        own = (p % 4 == 3)
        i = p // 4
        s_ = p % 2
        N = lambda base: base + str(s_)
        bu = bank(2)
        for c in range(2):
            for h in range(4):
                mm(psv(bu[c], h * 128, (h + 1) * 128), ktil[s_][64 * c:64 * c + 64, h * 128:(h + 1) * 128],
                   vbf[s_][64 * c:64 * c + 64, h * 128:(h + 1) * 128], True, True, [N("ktil"), N("vbf")], [R(bu[c])])
        bt = bank()[0]
        for hp in range(4):
            tr(psb(bt, hp * 128, (hp + 1) * 128), knb[s_][:, hp * 128:(hp + 1) * 128], ident_b, [N("knb"), "ident_b"], [R(bt)])
        kt_ = kTt[s_]
        act(kt_, psb(bt, 0, 512).rearrange("p (a b) -> p a b", a=4), AF.Copy, [R(bt)], [N("kTt")])
        P.dma("sp", N("kTw"), kT_scr.ap().rearrange("(a r) s -> r a s", a=4)[:, :, p * 128:(p + 1) * 128], kt_,
              reads=[N("kTt")], writes=["kT_scr"])
        bt2 = bank()[0]
        tr(psb(bt2, 0, 128), ikn2[s_], ident_b, [N("ikn2"), "ident_b"], [R(bt2)])
        act(ikT[:, p * 128:(p + 1) * 128], psb(bt2, 0, 128), AF.Copy, [R(bt2)], ["ikT"])
        e3 = ecols[s_].rearrange("p (h x) -> p h x", h=4)
        for c in range(2):
            if own:
                tt("dve", Sm[c], Sst, e3[:, :, 3 * c:3 * c + 1].to_broadcast([128, 4, 128]), ALU.mult,
                   ["S", N("ecols")], ["Sm%d" % c])
            tt("dve", tmp4, psv(bu[c]).rearrange("p (h x) -> p h x", h=4),
               e3[:, :, 3 * c + 2:3 * c + 3].to_broadcast([128, 4, 128]), ALU.mult, [R(bu[c]), N("ecols")], ["tmp4"])
            tt("pool", Sst, Sst, e3[:, :, 3 * c + 1:3 * c + 2].to_broadcast([128, 4, 128]), ALU.mult, ["S", N("ecols")], ["S"])
            tt("pool", Sst, Sst, tmp4, ALU.add, ["S", "tmp4"], ["S"])
        if not own:
            return
        tt("pool", qtil, f_q, f_e, ALU.mult, ["f_q", "f_e"], ["qtil"])
        bt3 = bank()[0]
        for h in range(4):
            tr(psb(bt3, h * 128, (h + 1) * 128), ktil[s_][:, h * 128:(h + 1) * 128], ident_b, [N("ktil"), "ident_b"], [R(bt3)])
        act(kq_T[:, 0:4, :], psb(bt3, 0, 512).rearrange("p (a b) -> p a b", a=4), AF.Copy, [R(bt3)], ["kq_Ta"])
        bt4 = bank()[0]
        for h in range(4):
            tr(psb(bt4, h * 128, (h + 1) * 128), qtil[:, h * 128:(h + 1) * 128], ident_b, ["qtil", "ident_b"], [R(bt4)])
        P.op("dve", lambda e, o=kq_T[:, 4:8, :], i_=psb(bt4, 0, 512).rearrange("p (a b) -> p a b", a=4): e.tensor_copy(o, i_),
             reads=[R(bt4)], writes=["kq_Tb"])
        bt6 = bank()[0]
        for hp in range(4):
            tr(psb(bt6, hp * 128, (hp + 1) * 128), iqb[:, hp * 128:(hp + 1) * 128], ident_b, ["iqb", "ident_b"], [R(bt6)])
        act(iqTt, psb(bt6, 0, 512).rearrange("p (a b) -> p a b", a=4), AF.Copy, [R(bt6)], ["iqTt"])
        P.dma("sp", "iqTw", iqT_scr.ap()[i * 128:(i + 1) * 128, :].rearrange("p (a t) -> p a t", a=4), iqTt,
              reads=["iqTt"], writes=["iqT_scr"])
        ba = bank()[0]
        for h in range(4):
            mm(psv(ba, h * 128, (h + 1) * 128), kq_T[:, h, :], kq_T[:, 4 + h, :], True, True, ["kq_Ta", "kq_Tb"], [R(ba)])
        tt("dve", A_T, psv(ba).rearrange("p (h x) -> p h x", h=4), hmask.unsqueeze(1).to_broadcast([128, 4, 128]),
           ALU.mult, [R(ba), "hmask"], ["A_T"])
        headnorm(f_aq, 8, 64, qg_bc, qtil, "f_aq", "q_norm_g", s_, dtag="qtil")
        bo = bank(2)
        for c in range(2):
            for h in range(4):
                mm(psv(bo[c], h * 128, (h + 1) * 128), A_T[:, h, :], vbf[s_][:, h * 128:(h + 1) * 128], True, False,
                   ["A_T", N("vbf")], [R(bo[c])])
                mm(psv(bo[c], h * 128, (h + 1) * 128), kq_T[:, 4 + h, :], Sm[c][:, h, :], False, True,
                   ["kq_Tb", "Sm%d" % c], [R(bo[c])])
        cp("dve", recraw[0:64, :], psv(bo[0])[0:64, :], [R(bo[0])], ["recraw"])
        cp("dve", recraw[64:128, :], psv(bo[1])[64:128, :], [R(bo[1])], ["recraw"])
        bt5 = bank()[0]
        for hp in range(4):
            tr(psb(bt5, hp * 128, (hp + 1) * 128), qtil[:, hp * 128:(hp + 1) * 128], ident_b, ["qtil", "ident_b"], [R(bt5)])
        act(qTt, psb(bt5, 0, 512).rearrange("p (a b) -> p a b", a=4), AF.Copy, [R(bt5)], ["qTt"])
        P.dma("sp", "qTw", qT_scr.ap()[i * 128:(i + 1) * 128, :].rearrange("p (a t) -> p a t", a=4), qTt,
              reads=["qTt"], writes=["qT_scr"])
        headnorm(recraw, 4, 128, recg_bc, recraw, "recraw", "rec_out_g", s_)
        tt("pool", recb, recraw, f_gate, ALU.mult, ["recraw", "f_gate"], ["recb"])
        P.dma("sp", "recw", rec_scr.ap()[i * 128:(i + 1) * 128, :], recb, reads=["recb"], writes=["rec_scr"])

    TPT = TTW // 128
    xload(0)
    xcast(0)
    early(0)
    late(0)
    for p in range(NPT):
        if p + 1 < NPT:
            if (p + 1) % TPT == 0:
                xcast((p + 1) // TPT)
            early(p + 1)
        late2(p)
        if p + 1 < NPT:
            late(p + 1)

    P.barrier()
    al2 = Alloc(A0, ARENA)
    _wA = al2([8, DIN], BF16); _cr = al2([DIN + 24], BF16, parts=1)
    IKT_OFF = al2.o
    _ik = al2([S], BF16)
    IKT_END = al2.o
    r1 = Alloc(A0, IKT_OFF)
    r2 = Alloc(IKT_END, ARENA)
    NVT = 4
    score = [r1([S], F32), r1([S], F32)]
    kbuf = [r1([4, NVT * 128], BF16), r1([4, NVT * 128], BF16)]
    mneg = [r2([S], BF16), r2([S], BF16)]
    vbuf = [r2([NVT, 520], BF16), r2([NVT, 520], BF16)]
    relu_b = [r2([512], BF16), r2([512], BF16), r2([512], BF16)]
    diagw = r2([8, 128], BF16)
    hank = r2([2, 8, 128], BF16)
    i30k4 = r2([512], BF16)
    iqz = [r2([8, 128], BF16), r2([8, 128], BF16)]
    qbd = [r2([4, 256], BF16), r2([4, 256], BF16)]
    pT = [r2([4, 128], BF16), r2([4, 128], BF16), r2([4, 128], BF16)]
    attraw = r2([512], F32)
    bis = [r2([32], F32), r2([32], F32)]
    hank_f = score[0][:, 0:2048].rearrange("p (a b) -> p a b", a=16)

    P.dma("sp", "c_i30k4", i30k4, T["i30k4_b"], writes=["i30k4"])
    for dl in range(2):
        for h in range(8):
            P.dma("sp", "hkl", hank_f[:, dl * 8 + h, :], bass.AP(fv_scr, h * 384 + (128 if dl == 1 else 0), [[1, 128], [1, 128]]),
                  reads=["fv_scr"], writes=["score0"])
    cp("dve", hank.rearrange("p a b c -> p (a b) c"), hank_f, ["score0"], ["hank"])
    ms("pool", qbd[0], 0.0, ["qbd0"])
    ms("pool", qbd[1], 0.0, ["qbd1"])
    ms("pool", iqz[0], 0.0, ["iqz0"])
    ms("pool", iqz[1], 0.0, ["iqz1"])

    OB = [6, 7]
    v_v = v_scr.ap().rearrange("(t s) c -> s t c", s=128)
    kT_v = kT_scr.ap().rearrange("(a r) s -> r a s", a=4)
    vchunks = [(i_, c_) for i_ in range(NB) for c_ in range((4 * (i_ + 1)) // NVT)]
    gcbase = [0]
    for i_ in range(NB):
        gcbase.append(gcbase[-1] + (4 * (i_ + 1)) // NVT)

    def vload(gc):
        i_, c_ = vchunks[gc]
        P.dma("sp", "vbuf%d" % (gc % 2), vbuf[gc % 2], v_v[:, c_ * NVT:(c_ + 1) * NVT, :], reads=["v_scr"], writes=["vbuf%d" % (gc % 2)])
        P.dma("sp", "kbuf%d" % (gc % 2), kbuf[gc % 2], kT_v[:, :, c_ * NVT * 128:(c_ + 1) * NVT * 128], reads=["kT_scr"], writes=["kbuf%d" % (gc % 2)])

    def indexer(i):
        G = i + 1
        par = i % 2
        iqn = "iqz%d" % par
        sc_ = score[par]
        scn = "score%d" % par
        iqv = iqT_scr.ap()[i * 128:(i + 1) * 128, :].rearrange("p (a t) -> p a t", a=4)
        iz4 = iqz[par].rearrange("p (a two) t -> p a two t", two=2)
        P.dma("sp", "iqTl%d" % par, iz4[0:64, :, 0, :], iqv[0:64], reads=["iqT_scr"], writes=[iqn])
        P.dma("sp", "iqTl%d" % par, iz4[64:128, :, 1, :], iqv[64:128], reads=["iqT_scr"], writes=[iqn])
        for h in range(8):
            ts("pool", diagw[:, h, :], ident_b, iw_all[:, i, h:h + 1], None, ALU.mult, ALU.bypass, ["ident_b", "iw%d" % i], ["diagw"])

        def qk_relu(g, h):
            bs = (g * 8 + h) % 4
            mm(psv(bs), iqz[par][:, h, :], ikT[:, g * 512:(g + 1) * 512], True, True, [iqn, "ikT"], [R(bs)])
            k3 = (g * 8 + h) % 3
            act(relu_b[k3], psv(bs), AF.Relu, [R(bs)], ["relu%d" % k3])

        def accum(g, h):
            bacc = 4 + (g % 2)
            k3 = (g * 8 + h) % 3
            rb = relu_b[k3]
            rn = "relu%d" % k3
            last = (h == 7) and (g != 0) and (g != G - 1)
            mm(psv(bacc), diagw[:, h, :], rb, h == 0, last, ["diagw", rn], [R(bacc)])
            if h == 7:
                if g == 0:
                    mm(psv(bacc), ones_row[0:1, :], dpen[0:1, :], False, g != G - 1, ["ones_row", "dpen"], [R(bacc)])
                if g == G - 1:
                    mm(psv(bacc), u64[0:1, :], penrow[0:1, :], False, True, ["u64", "penrow"], [R(bacc)])
                act(sc_[:, g * 512:(g + 1) * 512], psv(bacc), AF.Relu, [R(bacc), "hank"], [scn], bias=64.0)

        prev = None
        for g in range(G):
            for h in range(8):
                qk_relu(g, h)
                if prev is not None:
                    accum(*prev)
                prev = (g, h)
        accum(*prev)

    def bisect(i):
        n = 512 * (i + 1)
        par = i % 2
        b_ = bis[par]
        sc_ = score[par]
        scn = "score%d" % par
        mid = b_[:, 1:2]; cnt = b_[:, 2:3]; tmpc = b_[:, 3:4]; lo = b_[:, 0:1]
        mn = mneg[par]
        mname = "mneg%d" % par
        bn = "bis%d" % par
        ms("dve", mid, 1.0 + 127.0 / 2, [bn])
        for it in range(NBIS):
            wk_ = 127.0 / (2 ** (it + 1))
            ts("dve", mn[:, 0:n], sc_[:, 0:n], mid, None, ALU.is_ge, ALU.add, [scn, bn], [mname, bn], accum_out=cnt)
            ts("dve", tmpc, cnt, 255.5, wk_, ALU.is_ge, ALU.mult, [bn], [bn])
            P.op("dve", lambda e, w=wk_, o=mid, t_=tmpc: e.scalar_tensor_tensor(o, t_, -w / 2, o, ALU.add, ALU.add), reads=[bn], writes=[bn])
        ts("dve", lo, mid, -127.0 / (2 ** (NBIS + 1)), None, ALU.add, ALU.bypass, [bn], [bn])
        ts("dve", mn[:, 0:n], sc_[:, 0:n], lo, 1.0, ALU.is_ge, ALU.subtract, [scn, bn], [mname])

    def attention(i):
        NKT = 4 * (i + 1)
        par = i % 2
        mn = mneg[par]
        mname = "mneg%d" % par
        qn = "qbd%d" % par
        qv = qT_scr.ap()[i * 128:(i + 1) * 128, :].rearrange("p (a t) -> p a t", a=4)
        P.dma("sp", "qbdl%d" % par, qbd[par][0:64, :, 0:128], qv[0:64], reads=["qT_scr"], writes=[qn])
        P.dma("sp", "qbdl%d" % par, qbd[par][64:128, :, 128:256], qv[64:128], reads=["qT_scr"], writes=[qn])
        steps = [(kt, hh) for kt in range(NKT) for hh in range(2)]
        info = {}

        def qk(kt, hh):
            near = kt >= NKT - 2
            dl = 0 if kt == NKT - 2 else 1
            gc = gcbase[i] + kt // NVT
            kb = kbuf[gc % 2]
            kr = "kbuf%d" % (gc % 2)
            bL = bank()[0]
            mm(psv(bL), mn[:, kt * 128:(kt + 1) * 128], i30k4, True, False, [mname, "i30k4"], [R(bL)])
            for pp in range(2):
                hp = hh * 2 + pp
                mm(psv(bL, pp * 256, (pp + 1) * 256), kb[:, hp, (kt % NVT) * 128:(kt % NVT + 1) * 128], qbd[par][:, hp, :], False,
                   (not near) and pp == 1, [kr, qn], [R(bL)])
            if near:
                for h4 in range(4):
                    h = hh * 4 + h4
                    mm(psv(bL, h4 * 128, (h4 + 1) * 128), hank[:, dl, h, :], jmat_b, False, h4 == 3, ["hank", "jmat_b"], [R(bL)])
            k3 = (kt * 2 + hh) % 3
            pt = pT[k3]
            pr = "pT%d" % k3
            act(pt, psv(bL).rearrange("p (a b) -> p a b", a=4), AF.Exp, [R(bL)], [pr])
            info[(kt, hh)] = (pt, pr)

        def pv(kt, hh):
            gc = gcbase[i] + kt // NVT
            if kt % NVT == 0 and hh == 0 and gc + 1 < len(vchunks):
                vload(gc + 1)
            vb = vbuf[gc % 2]
            vr = "vbuf%d" % (gc % 2)
            pt, pr = info[(kt, hh)]
            for h4 in range(4):
                h = hh * 4 + h4
                mm(psv(OB[hh], h4 * 128, h4 * 128 + 65), pt[:, h4, :], vb[:, kt % NVT, h * 65:(h + 1) * 65],
                   kt == 0 and h4 == 0, kt == NKT - 1, [pr, vr], [R(OB[hh])], skip=True)

        qk(*steps[0])
        for si in range(len(steps)):
            if si + 1 < len(steps):
                qk(*steps[si + 1])
            pv(*steps[si])

    def attnorm(i):
        b_ = bis[i % 2]
        den = b_[:, 4:12]
        for hh in range(2):
            o3 = psv(OB[hh]).rearrange("p (a b) -> p a b", a=4)
            P.op("dve", lambda e, o=den[:, hh * 4:hh * 4 + 4], i_=o3[:, :, 64]: e.reciprocal(o, i_), reads=[R(OB[hh])], writes=["den%d%d" % (i % 2, hh)])
            tt("dve", attraw[:, hh * 256:(hh + 1) * 256].rearrange("p (a b) -> p a b", a=4), o3[:, :, 0:64],
               den[:, hh * 4:hh * 4 + 4].unsqueeze(2).to_broadcast([128, 4, 64]), ALU.mult, [R(OB[hh]), "den%d%d" % (i % 2, hh)], ["attraw"])
        sqb = pT[0].rearrange("p a b -> p (a b)")
        act(sqb, attraw, AF.Square, ["attraw"], ["pT0"])
        ss8 = b_[:, 12:20]
        sn = "ss8%d" % (i % 2)
        P.op("dve", lambda e, o=ss8, i_=sqb.rearrange("p (h d) -> p h d", h=8): e.tensor_reduce(o, i_, AX.X, ALU.add), reads=["pT0"], writes=[sn])
        rsqrt_small(sn, ss8, ss8, 1.0 / 64, [sn])
        a3 = attraw.rearrange("p (h d) -> p h d", h=8)
        tt("dve", a3, a3, ss8.unsqueeze(2).to_broadcast([128, 8, 64]), ALU.mult, ["attraw", sn], ["attraw"])
        tt("pool", att_all[:, i, :], attraw, attg_bc, ALU.mult, ["attraw", "attn_out_g"], ["att_all%d" % i])

    bank_state["set"] = list(range(4))
    bank_state["i"] = 0
    vload(0)
    indexer(0)
    bisect(0)
    indexer(1)
    for i in range(NB):
        if i + 2 < NB:
            indexer(i + 2)
        attention(i)
        if i + 1 < NB:
            bisect(i + 1)
        attnorm(i)

    if DEBUG:
        P.barrier()
        dacc = view(A0, [NB, D], F32)
        for i in range(NB):
            P.dma("sp", "recl", dacc[:, i, 0:256].bitcast(BF16) if False else view(A0 + 80 * 1024, [512], BF16), rec_scr.ap()[i * 128:(i + 1) * 128, :], reads=["rec_scr"], writes=["recd"])
            cp("dve", dacc[:, i, 0:512], view(A0 + 80 * 1024, [512], BF16), ["recd"], ["dacc%d" % i])
            cp("dve", dacc[:, i, 512:1024], att_all[:, i, :], ["att_all%d" % i], ["dacc%d" % i])
            P.dma("sp", "outw", out_d.rearrange("(i p) d -> p i d", p=128)[:, i, :], dacc[:, i, :], reads=["dacc%d" % i], writes=["out"])
        P.final_wait("sp", ["out"])
        P.emit()
        return nc

    PHB = ["kT_res", "score", "mneg", "vbuf0", "vbuf1", "relu0", "relu1", "relu2", "diagw", "hank", "qTb", "iqTb",
           "pT0", "pT1", "pT2", "attraw0", "attraw1", "ikT", "ss8", "rs8", "lo", "mid", "cnt", "ge", "den0", "den1"]
    P.barrier()
    bank_state["set"] = list(range(8))
    bank_state["i"] = 0
    comb_scr = nc.dram_tensor("comb_scr", [16, NB * 128], F32, kind="Internal")
    cl = Alloc(A0, ARENA)
    acc = cl([NB, D], F32)
    h2T = cl([8, NB * 128], BF16)
    WX0 = cl.o
    wexp = [[cl([8, 512], BF16), cl([8, 512], BF16), cl([4, D], BF16)] for _ in range(2)]
    WX1 = cl.o
    stg = [cl([1024], F32), cl([1024], F32)]
    wr_b = cl([8, 20], BF16)
    wr_f = cl([8, 20], F32)
    brbc = cl([20], F32)
    rt2 = [cl([128], F32), cl([128], F32)]
    cstage = cl([128], F32)
    recc = [cl([512], BF16), cl([512], BF16)]
    he = [cl([4, 512], BF16), cl([4, 512], BF16)]
    cbt = [cl([512], F32), cl([512], F32)]
    s1t = [cl([512], BF16), cl([512], BF16)]
    t3t = [cl([512], BF16), cl([512], BF16)]
    g2bc = cl([D], F32)
    C_END = cl.o
    c1 = Alloc(WX0, WX1)
    wout_b = c1([8, D], BF16)
    g1bc = c1([D], F32); a2bc = c1([D], F32); sh2bc = c1([D], F32)
    mixT = [c1([8, 128], BF16), c1([8, 128], BF16)]
    tmpf = [c1([D], F32), c1([D], F32)]
    h2b = [c1([D], BF16), c1([D], BF16)]
    C1RES = ["wout_b", "g1bc", "a2bc", "sh2bc", "mixT", "tmpf", "h2b"]

    first = PHB
    modb = lambda dst, k, name: P.dma("sp", "c_" + name, dst, modv[:, k * D:(k + 1) * D].to_broadcast([128, D]),
                                      reads=["mod_scr"], writes=[name] + first)
    modb(g1bc, 2, "g1bc"); modb(sh2bc, 3, "sh2bc"); modb(a2bc, 4, "a2bc")
    P.dma("sp", "c_n2g", tmpf[0], T["n2g"].to_broadcast([128, D]), writes=["tmpf0"] + first)
    ts("dve", a2bc, a2bc, 1.0, None, ALU.add, ALU.bypass, ["a2bc"], ["a2bc"])
    tt("dve", a2bc, a2bc, tmpf[0], ALU.mult, ["a2bc", "tmpf0"], ["a2bc"])
    P.dma("sp", "c_wr", wr_f, T["w_r"].rearrange("(k p) c -> p k c", p=128), writes=["wr_f"] + first)
    cp("dve", wr_b, wr_f, ["wr_f"], ["wr_b"] + first)
    P.dma("sp", "c_br", brbc, T["b_r"].to_broadcast([128, 20]), writes=["brbc"] + first)
    wo_v = T["w_out"].rearrange("(k p) c -> p k c", p=128)
    for k in range(8):
        sb = stg[k % 2]
        P.dma("sp", "stg%d" % (k % 2), sb, wo_v[:, k, :], writes=["stg%d" % (k % 2)] + (first if k < 2 else []))
        cp("pool" if k % 2 == 0 else "dve", wout_b[:, k, :], sb, ["stg%d" % (k % 2)], ["wout_b"] + (first if k == 0 else []))
    xo_v = T["x_own"].rearrange("(i p) d -> p i d", p=128)
    for i in range(NB):
        P.dma("sp", "xo", acc[:, i, :], xo_v[:, i, :], writes=["acc%d" % i] + (first if i == 0 else []))

    def recload(i_):
        P.dma("sp", "recc%d" % (i_ % 2), recc[i_ % 2], rec_scr.ap()[i_ * 128:(i_ + 1) * 128, :], reads=["rec_scr"], writes=["recc%d" % (i_ % 2)])

    def c1_main(i):
        s_ = i % 2
        N = lambda b_: b_ + str(s_)
        tf = tmpf[s_]
        if i + 1 < NB:
            recload(i + 1)
        bt = bank()[0]
        for k in range(8):
            src = recc[s_][:, k * 128:(k + 1) * 128] if k < 4 else att_all[:, i, (k - 4) * 128:(k - 3) * 128]
            tr(psb(bt, k * 128, (k + 1) * 128), src, ident_b, [N("recc"), "att_all%d" % i, "ident_b"], [R(bt)])
        act(mixT[s_], psb(bt).rearrange("p (a b) -> p a b", a=8), AF.Copy, [R(bt)], [N("mixT")])
        for half in range(2):
            b = bank()[0]
            for k in range(8):
                mm(psv(b), mixT[s_][:, k, :], wout_b[:, k, half * 512:(half + 1) * 512], k == 0, k == 7, [N("mixT"), "wout_b"], [R(b)])
            tt("dve", tf[:, half * 512:(half + 1) * 512], psv(b), g1bc[:, half * 512:(half + 1) * 512], ALU.mult, [R(b), "g1bc", "a2bc"], [N("tmpf")])
        tt("dve", acc[:, i, :], acc[:, i, :], tf, ALU.add, ["acc%d" % i, N("tmpf")], ["acc%d" % i])
        rt = rt2[s_]
        ss = rt[:, 0:1]
        act(tf, acc[:, i, :], AF.Square, ["acc%d" % i], [N("tmpf"), N("ss2")], accum_out=ss)
        rsqrt_small(N("ss2"), ss, ss, 1.0 / D, [N("ss2")])
        act(tf, acc[:, i, :], AF.Identity, ["acc%d" % i, N("ss2")], [N("tmpf")], scale=ss)
        tt("dve", tf, tf, a2bc, ALU.mult, [N("tmpf"), "a2bc"], [N("tmpf")])
        tt("dve", h2b[s_], tf, sh2bc, ALU.add, [N("tmpf"), "sh2bc"], [N("h2b")])
        bt = bank()[0]
        for k in range(8):
            tr(psb(bt, k * 128, (k + 1) * 128), h2b[s_][:, k * 128:(k + 1) * 128], ident_b, [N("h2b"), "ident_b"], [R(bt)])
        act(h2T[:, :, i * 128:(i + 1) * 128], psb(bt).rearrange("p (a b) -> p a b", a=8), AF.Copy, [R(bt)], ["h2T%d" % i])
        b = bank()[0]
        for k in range(8):
            mm(psv(b, 0, 20), h2T[:, k, i * 128:(i + 1) * 128], wr_b[:, k, :], k == 0, k == 7, ["h2T%d" % i, "wr_b"], [R(b)])
        lg = rt[:, 4:24]
        tt("dve", lg, psv(b, 0, 20), brbc, ALU.add, [R(b), "brbc"], [N("lg")])

    def c1_router(i):
        s_ = i % 2
        N = lambda b_: b_ + str(s_)
        rt = rt2[s_]
        lg = rt[:, 4:24]
        gmax = rt[:, 24:25]; ngmax = rt[:, 25:26]; sumg = rt[:, 26:27]; eg = rt[:, 28:32]
        P.op("dve", lambda e, o=gmax, i_=lg[:, 0:4]: e.tensor_reduce(o, i_, AX.X, ALU.max), reads=[N("lg")], writes=[N("gmax")])
        ts("dve", ngmax, gmax, -1.0, None, ALU.mult, ALU.bypass, [N("gmax")], [N("ngmax")])
        act(eg, lg[:, 0:4], AF.Exp, [N("lg"), N("ngmax")], [N("eg"), N("sumg")], bias=ngmax, accum_out=sumg)
        gate = rt[:, 27:28]
        P.op("dve", lambda e, o=gate, i_=sumg: e.reciprocal(o, i_), reads=[N("sumg")], writes=[N("gate")])
        ohg = rt[:, 32:36]
        ts("dve", ohg, lg[:, 0:4], gmax, None, ALU.is_ge, ALU.bypass, [N("lg"), N("gmax")], [N("ohg")])
        ts("dve", ohg, ohg, 1.0, 1.0e4, ALU.subtract, ALU.mult, [N("ohg")], [N("ohg")])
        em = rt[:, 36:52]
        tt("dve", em.rearrange("p (g e) -> p g e", g=4), lg[:, 4:20].rearrange("p (g e) -> p g e", g=4),
           ohg.unsqueeze(2).to_broadcast([128, 4, 4]), ALU.add, [N("lg"), N("ohg")], [N("em")])
        top8 = rt[:, 52:60]
        P.op("dve", lambda e, o=top8, i_=em: e.max(o, i_), reads=[N("em")], writes=[N("top8")])
        nv1 = rt[:, 60:61]
        ts("dve", nv1, top8[:, 0:1], -1.0, None, ALU.mult, ALU.bypass, [N("top8")], [N("nv1")])
        sel = rt[:, 64:80]
        ts("dve", sel, em, top8[:, 1:2], None, ALU.is_ge, ALU.bypass, [N("em"), N("top8")], [N("sel")])
        ex = rt[:, 80:96]
        act(ex, em, AF.Exp, [N("em"), N("nv1")], [N("ex")], bias=nv1)
        e2 = rt[:, 61:62]
        act(e2, top8[:, 1:2], AF.Exp, [N("top8"), N("nv1")], [N("e2")], bias=nv1)
        ts("dve", e2, e2, 1.0, None, ALU.add, ALU.bypass, [N("e2")], [N("e2")])
        P.op("dve", lambda e, o=e2: e.reciprocal(o, o), reads=[N("e2")], writes=[N("e2")])
        tt("dve", e2, e2, gate, ALU.mult, [N("e2"), N("gate")], [N("e2")])
        tt("dve", ex, ex, sel, ALU.mult, [N("ex"), N("sel")], [N("ex")])
        comb = rt[:, 96:112]
        ts("dve", comb, ex, e2, None, ALU.mult, ALU.bypass, [N("ex"), N("e2")], [N("comb")])
        b = bank()[0]
        mm(psv(b, 0, 128)[0:16, :], comb, ident_f, True, True, [N("comb"), "ident_f"], [R(b)])
        cp("dve", cstage[0:16, :], psv(b, 0, 128)[0:16, :], [R(b)], ["cstage"])
        P.dma("pool", "combw", comb_scr.ap()[:, i * 128:(i + 1) * 128], cstage[0:16, :], reads=["cstage"], writes=["comb_scr"])

    recload(0)
    c1_main(0)
    for i in range(NB):
        if i + 1 < NB:
            c1_main(i + 1)
        c1_router(i)

    w1_v = T["w1"].rearrange("(e k p) f -> e p k f", e=16, p=128)
    w3_v = T["w3"].rearrange("(e k p) f -> e p k f", e=16, p=128)
    w2_v = T["w2"].rearrange("(e k p) d -> e p k d", e=16, p=128)
    sidx = [0]
    ALLRA = []
    P.dma("sp", "c_g2bc", g2bc, modv[:, 5 * D:6 * D].to_broadcast([128, D]), reads=["mod_scr"], writes=["g2bc"] + ALLRA)

    def stage_cast(src_ap, dst, rname, a, g2=False, extra=()):
        s_ = sidx[0] % 2
        sidx[0] += 1
        sb = stg[s_].rearrange("p (a b) -> p a b", a=a)
        P.dma("sp", "stg%d" % s_, sb, src_ap, writes=["stg%d" % s_])
        eng_ = "pool" if s_ == 0 else "dve"
        if g2:
            tt(eng_, dst, sb, g2bc.unsqueeze(1).to_broadcast([128, a, D]), ALU.mult, ["stg%d" % s_, "g2bc"], [rname] + list(extra))
        else:
            cp(eng_, dst, sb, ["stg%d" % s_], [rname] + list(extra))

    def stage_expert(e_):
        wb = wexp[e_ % 2]
        wn = "wexp%d" % (e_ % 2)
        for kk in range(4):
            stage_cast(w1_v[e_][:, kk * 2:(kk + 1) * 2, :], wb[0][:, kk * 2:(kk + 1) * 2, :], wn + "a", 2)
        for kk in range(4):
            stage_cast(w3_v[e_][:, kk * 2:(kk + 1) * 2, :], wb[1][:, kk * 2:(kk + 1) * 2, :], wn + "b", 2)
        for kk in range(4):
            stage_cast(w2_v[e_][:, kk:kk + 1, :], wb[2][:, kk:kk + 1, :], wn + "c", 1, g2=True)

    def moe_H(e_, tg):
        wb = wexp[e_ % 2]
        wn = "wexp%d" % (e_ % 2)
        allh = ["h2T%d" % (tg * 4 + x) for x in range(4)]
        ci = (e_ * 4 + tg) % 2
        cb_ = cbt[ci]
        cn = "cbt%d" % ci
        P.dma("act", cn, cb_, comb_scr.ap()[e_:e_ + 1, tg * 512:(tg + 1) * 512].to_broadcast([128, 512]), reads=["comb_scr"], writes=[cn])
        hb_ = he[ci]
        hn = "he%d" % ci
        for ft in range(4):
            b1 = bank()[0]
            for k in range(8):
                mm(psv(b1), wb[0][:, k, ft * 128:(ft + 1) * 128], h2T[:, k, tg * 512:(tg + 1) * 512], k == 0, k == 7, [wn + "a"] + allh, [R(b1)])
            b3 = bank()[0]
            for k in range(8):
                mm(psv(b3), wb[1][:, k, ft * 128:(ft + 1) * 128], h2T[:, k, tg * 512:(tg + 1) * 512], k == 0, k == 7, [wn + "b"] + allh, [R(b3)])
            s1 = s1t[ft % 2]; t3 = t3t[ft % 2]
            act(s1, psv(b1), AF.Silu, [R(b1)], ["s1%d" % (ft % 2)])
            tt("dve", t3, psv(b3), cb_, ALU.mult, [R(b3), cn], ["t3%d" % (ft % 2)])
            tt("pool", hb_[:, ft, :], s1, t3, ALU.mult, ["s1%d" % (ft % 2), "t3%d" % (ft % 2)], [hn])

    def moe_Y(e_, tg):
        wb = wexp[e_ % 2]
        wn = "wexp%d" % (e_ % 2)
        ci = (e_ * 4 + tg) % 2
        hb_ = he[ci]
        hn = "he%d" % ci
        for tb in range(4):
            i = tg * 4 + tb
            for half in range(2):
                by = bank()[0]
                for ft in range(4):
                    mm(psv(by), hb_[:, ft, tb * 128:(tb + 1) * 128], wb[2][:, ft, half * 512:(half + 1) * 512], ft == 0, ft == 3,
                       [hn, wn + "c"], [R(by)])
                tt("dve", acc[:, i, half * 512:(half + 1) * 512], acc[:, i, half * 512:(half + 1) * 512], psv(by), ALU.add,
                   [R(by), "acc%d" % i], ["acc%d" % i])

    P.barrier()
    stage_expert(0)
    stage_expert(1)
    msteps = [(e_, tg) for e_ in range(16) for tg in range(4)]
    moe_H(*msteps[0])
    for si in range(len(msteps)):
        if si + 1 < len(msteps):
            moe_H(*msteps[si + 1])
        moe_Y(*msteps[si])
        e_, tg = msteps[si]
        if tg == 3 and e_ + 2 < 16:
            stage_expert(e_ + 2)

    out_v = out_d.rearrange("(i p) d -> p i d", p=128)
    for i in range(NB):
        P.dma("sp", "outw", out_v[:, i, :], acc[:, i, :], reads=["acc%d" % i], writes=["out"])
    P.final_wait("sp", ["out"])
    P.emit()
    return nc


_CACHE = {}


def kernel(**inputs):
    x = np.asarray(inputs["x"], np.float32)
    B = x.shape[0]
    f32 = lambda a: np.ascontiguousarray(np.asarray(a, np.float32))
    consts = _consts()
    shared = {
        "w_ada": f32(inputs["w_ada"][0]), "b_ada": f32(inputs["b_ada"][0])[None, :],
        "n1g_col": f32(np.asarray(inputs["norm1_g"][0]).reshape(8, 128).T), "n2g": f32(inputs["norm2_g"][0])[None, :],
        "w_in": f32(inputs["w_in"][0]), "lb_logits": f32(inputs["lb_logits"]),
        "rec_out_g": f32(inputs["rec_out_g"][0])[None, :], "q_norm_g": f32(inputs["q_norm_g"][0])[None, :],
        "k_norm_g": f32(inputs["k_norm_g"][0])[None, :], "ikg": f32(inputs["idx_k_norm_g"][0])[None, :],
        "ikb": f32(inputs["idx_k_norm_b"][0])[None, :], "attn_out_g": f32(inputs["attn_out_g"][0])[None, :],
        "rel_bias": f32(inputs["rel_bias"]), "w_out": f32(inputs["w_out"][0]),
        "w_r": f32(np.concatenate([np.asarray(inputs["w_rg"][0]), np.asarray(inputs["w_re"][0])], axis=1)),
        "b_r": f32(np.concatenate([np.asarray(inputs["b_rg"][0]), np.asarray(inputs["b_re"][0])]))[None, :],
        "w1": f32(np.asarray(inputs["w1"][0]).reshape(16 * D, 512)), "w3": f32(np.asarray(inputs["w3"][0]).reshape(16 * D, 512)),
        "w2": f32(np.asarray(inputs["w2"][0]).reshape(16 * 512, D)),
    }
    shared.update(consts)
    c = np.asarray(inputs["c"], np.float32)
    in_maps = []
    for core in range(8):
        b, j = core // 4, core % 4
        off = (3 - j) * 128
        xT = np.zeros((D, S), np.float32)
        xT[:, off:] = x[b, :S - off, :].T
        valid = np.ones((S,), np.float32)
        valid[:off] = 0.0
        dp = np.zeros((1, 512), np.float32)
        dp[0, :off] = -BIG
        own = np.concatenate([x[b, (4 * i + j) * 128:(4 * i + j + 1) * 128, :] for i in range(NB)], axis=0)
        m = dict(shared)
        m.update({
            "xT": xT, "x_own": f32(own), "valid": f32(valid.reshape(NPT, 128).T), "dpen": dp.astype(ml_dtypes.bfloat16),
            "c_col": f32(c[b].reshape(8, 128).T),
        })
        in_maps.append(m)
    if "nc" not in _CACHE:
        _CACHE["nc"] = build_program()
    nc = _CACHE["nc"]
    res = run_bass_kernel_spmd(nc, in_maps, core_ids=list(range(8)))
    out = np.zeros((B, S, D), np.float32)
    for core in range(8):
        b, j = core // 4, core % 4
        o = np.asarray(res.results[core]["out"], np.float32)
        for i in range(NB):
            out[b, (4 * i + j) * 128:(4 * i + j + 1) * 128, :] = o[i * 128:(i + 1) * 128, :]
    return out
```

```python
import contextlib
import numpy as np
import ml_dtypes
import concourse.bass as bass
import concourse.mybir as mybir
from concourse.bass_utils import run_bass_kernel_spmd

F32 = mybir.dt.float32
BF16 = mybir.dt.bfloat16
ALU = mybir.AluOpType
AF = mybir.ActivationFunctionType
AX = mybir.AxisListType

EPOCH = 12000
D = 1024
S = 8192
NPT = 64
NTT = 32
TTW = 256
NB = 16
DIN = 4168
EPS = 1e-6
BIG = 30000.0
NBIS = 19
DEBUG = False


class Prog:
    ENG = ("pe", "act", "dve", "pool", "sp")

    def __init__(self, nc):
        self.nc = nc
        self.ops = {e: [] for e in self.ENG}
        self.cnt = {e: 0 for e in self.ENG}
        self.seen = {e: {} for e in self.ENG}
        self.last_w = {}
        self.readers = {}
        self.dma_cnt = {}
        self.fence = {}

    def barrier(self):
        f = {}
        for e in self.ENG:
            n = self.cnt[e]
            if n > 0:
                ep = (n - 1) // EPOCH
                f[("e", e, ep)] = n - ep * EPOCH
        for name, k in self.dma_cnt.items():
            f[("d", name)] = k
        self.fence = f

    def _eng_clock(self, eng):
        n = self.cnt[eng] + 1
        ep = (n - 1) // EPOCH
        return ("e", eng, ep), n - ep * EPOCH

    def _deps(self, eng, reads, writes):
        deps = {}

        def add(cv):
            if cv is None:
                return
            c, v = cv
            if c[0] == "d":
                v = self.dma_cnt[c[1]]
            if deps.get(c, 0) < v:
                deps[c] = v
        for r in reads:
            add(self.last_w.get(r))
        for w in writes:
            add(self.last_w.get(w))
            for c, v in self.readers.get(w, {}).items():
                add((c, v))
        for c, v in self.fence.items():
            if self.seen[eng].get(c, 0) < v:
                add((c, v))
        out = []
        for c, v in deps.items():
            if c[0] == "e" and c[1] == eng and eng == "pe":
                continue
            if self.seen[eng].get(c, 0) >= v:
                continue
            self.seen[eng][c] = v
            out.append((c, v))
        return out

    def _mark(self, clock, val, reads, writes):
        for r in reads:
            d = self.readers.setdefault(r, {})
            if d.get(clock, 0) < val:
                d[clock] = val
        for w in writes:
            self.last_w[w] = (clock, val)
            self.readers[w] = {}

    def op(self, eng, fn, reads=(), writes=()):
        waits = self._deps(eng, reads, writes)
        clock, val = self._eng_clock(eng)
        self.cnt[eng] += 1
        self.ops[eng].append(("op", waits, fn, clock, val))
        self._mark(clock, val, reads, writes)

    def dma(self, q, semname, out, in_, reads=(), writes=(), **kw):
        waits = self._deps(q, reads, writes)
        k = self.dma_cnt.get(semname, 0) + 16
        self.dma_cnt[semname] = k
        clock = ("d", semname)
        self.ops[q].append(("dma", waits, (out, in_, kw), clock, k))
        self._mark(clock, k, reads, writes)

    def final_wait(self, eng, resources):
        waits = self._deps(eng, resources, ())
        self.ops[eng].append(("wait", waits, None, None, None))

    def emit(self):
        nc = self.nc
        clocks = set()
        for e in self.ENG:
            for o in self.ops[e]:
                for c, v in o[1]:
                    clocks.add(c)
                if o[3] is not None:
                    clocks.add(o[3])
        clocks = sorted(clocks, key=str)
        with contextlib.ExitStack() as st:
            sems = {}
            for i, c in enumerate(clocks):
                sems[c] = st.enter_context(nc.semaphore("s%d" % i))
            block = st.enter_context(nc.Block())

            def run(engname, eng):
                for kind, waits, payload, clock, val in self.ops[engname]:
                    for c, v in waits:
                        eng.wait_ge(sems[c], v)
                    if kind == "op":
                        payload(eng).then_inc(sems[clock], 1)
                    elif kind == "dma":
                        out, in_, kw = payload
                        eng.dma_start(out=out, in_=in_, **kw).then_inc(sems[clock], 16)

            @block.tensor
            def _(e):
                run("pe", e)

            @block.scalar
            def _(e):
                run("act", e)

            @block.vector
            def _(e):
                run("dve", e)

            @block.gpsimd
            def _(e):
                run("pool", e)

            @block.sync
            def _(e):
                run("sp", e)


def _t5_onehot():
    import jax
    import jax.numpy as jnp
    with jax.default_device(jax.devices("cpu")[0]):
        rel = jnp.arange(384, dtype=jnp.int32) - 255
        nb = 16
        max_exact = 8
        ret = jnp.where(rel > 0, nb, 0)
        n = jnp.abs(rel)
        nf = jnp.maximum(n, 1).astype(jnp.float32)
        large = max_exact + (jnp.log(nf / max_exact) / np.log(128 / max_exact)
                             * (nb - max_exact)).astype(jnp.int32)
        large = jnp.minimum(large, nb - 1)
        bucket = np.asarray(ret + jnp.where(n < max_exact, n, large))
    oh = np.zeros((32, 384), np.float32)
    oh[bucket, np.arange(384)] = 1.0
    oh[15, :] -= 1.0
    return oh


def _consts():
    c = {}
    idx = np.arange(128)
    c["ident_b"] = np.eye(128, dtype=np.float32).astype(ml_dtypes.bfloat16)
    c["ident_f"] = np.eye(128, dtype=np.float32)
    c["i30k_b"] = (np.eye(128, dtype=np.float32) * BIG).astype(ml_dtypes.bfloat16)
    c["i30k4_b"] = np.tile(np.eye(128, dtype=np.float32) * BIG, (1, 4)).astype(ml_dtypes.bfloat16)
    c["jmat_b"] = np.eye(128, dtype=np.float32)[::-1].copy().astype(ml_dtypes.bfloat16)
    ch = idx // 64
    mid = ch * 64 + 31
    same = ch[:, None] == ch[None, :]
    tri = (idx[:, None] <= idx[None, :]) & same
    tm = (idx[:, None] <= mid[None, :]) & same
    c["d1"] = (tri.astype(np.float32) - tm.astype(np.float32))
    cm = np.zeros((128, 6), np.float32)
    for cc in range(2):
        inc = ch == cc
        cm[:, 3 * cc + 0] = inc & (idx <= cc * 64 + 31)
        cm[:, 3 * cc + 1] = inc
        cm[:, 3 * cc + 2] = inc & (idx > cc * 64 + 31)
    c["cm"] = cm
    c["hmask"] = (same & (idx[None, :] >= idx[:, None])).astype(np.float32)
    c["oh"] = _t5_onehot()
    u64 = np.zeros((1, 128), np.float32)
    u64[0, :64] = 1.0
    c["u64"] = u64.astype(ml_dtypes.bfloat16)
    pen = np.zeros((1, 512), np.float32)
    pen[0, 448:] = -BIG
    c["penrow"] = pen.astype(ml_dtypes.bfloat16)
    sel = np.zeros((16, 16, 128), np.float32)
    for e in range(16):
        sel[e, e, :] = 1.0
    c["sel"] = sel.transpose(1, 0, 2).reshape(16, 16 * 128).astype(ml_dtypes.bfloat16)
    return c


CONST_SPECS = [
    ("ident_b", [128, 128], BF16), ("ident_f", [128, 128], F32), ("i30k_b", [128, 128], BF16), ("i30k4_b", [128, 512], BF16),
    ("jmat_b", [128, 128], BF16), ("d1", [128, 128], F32), ("cm", [128, 6], F32),
    ("hmask", [128, 128], F32), ("oh", [32, 384], F32), ("u64", [1, 128], BF16),
    ("penrow", [1, 512], BF16), ("sel", [16, 2048], BF16),
]

IN_SPECS = [
    ("xT", [D, S], F32), ("x_own", [NB * 128, D], F32), ("valid", [128, NPT], F32), ("dpen", [1, 512], BF16),
    ("c_col", [128, 8], F32), ("w_ada", [D, 6 * D], F32), ("b_ada", [1, 6 * D], F32),
    ("n1g_col", [128, 8], F32), ("n2g", [1, D], F32), ("w_in", [D, DIN], F32), ("lb_logits", [2, 512], F32),
    ("rec_out_g", [1, 512], F32), ("q_norm_g", [1, 64], F32), ("k_norm_g", [1, 64], F32),
    ("ikg", [1, 64], F32), ("ikb", [1, 64], F32), ("attn_out_g", [1, 512], F32), ("rel_bias", [32, 8], F32),
    ("w_out", [D, D], F32), ("w_r", [D, 20], F32), ("b_r", [1, 20], F32),
    ("w1", [16 * D, 512], F32), ("w3", [16 * D, 512], F32), ("w2", [16 * 512, D], F32),
]


def build_program():
    nc = bass.Bass("TRN2", target_bir_lowering=False)
    P = Prog(nc)
    T = {}
    for name, shape, dt in IN_SPECS + CONST_SPECS:
        T[name] = nc.dram_tensor(name, shape, dt, kind="ExternalInput").ap()
    out_d = nc.dram_tensor("out", [NB * 128, D], F32, kind="ExternalOutput").ap()
    dbg = {}

    mod_scr = nc.dram_tensor("mod_scr", [1, 6 * D], F32, kind="Internal")
    kT_scr = nc.dram_tensor("kT_scr", [4 * 128, S], BF16, kind="Internal")
    v_scr = nc.dram_tensor("v_scr", [S, 8 * 65], BF16, kind="Internal")
    qT_scr = nc.dram_tensor("qT_scr", [NB * 128, 4 * 128], BF16, kind="Internal")
    iqT_scr = nc.dram_tensor("iqT_scr", [NB * 128, 4 * 128], BF16, kind="Internal")
    fv_scr = nc.dram_tensor("fv_scr", [8, 384], F32, kind="Internal")
    rec_scr = nc.dram_tensor("rec_scr", [NB * 128, 512], BF16, kind="Internal")

    ARENA = 212480
    arena = nc.alloc_sbuf_tensor("arena", [128, ARENA // 4], F32)

    def view(off, shape, dt, parts=128):
        esz = 2 if dt == BF16 else 4
        n = int(np.prod(shape))
        assert off % 4 == 0 and off + n * esz <= ARENA, (off, shape)
        a = arena[0:parts, off // 4: off // 4 + (n * esz + 3) // 4]
        if dt == BF16:
            a = a.bitcast(BF16)[:, 0:n]
        if len(shape) == 2:
            a = a.rearrange("p (a b) -> p a b", a=shape[0])
        elif len(shape) == 3:
            a = a.rearrange("p (a b c) -> p a b c", a=shape[0], b=shape[1])
        return a

    class Alloc:
        def __init__(self, base, limit):
            self.o = base
            self.limit = limit

        def __call__(self, shape, dt, parts=128):
            esz = 2 if dt == BF16 else 4
            n = int(np.prod(shape)) * esz
            n = (n + 31) // 32 * 32
            v = view(self.o, shape, dt, parts)
            self.o += n
            assert self.o <= self.limit, (self.o, self.limit)
            return v

    psall = nc.alloc_psum_tensor("psall", [128, 4096], F32)
    bank_state = {"i": 0, "set": list(range(8))}

    def bank(n=1):
        s = bank_state["set"]
        while True:
            i = bank_state["i"] % len(s)
            ids = s[i:i + n]
            bank_state["i"] += 1
            if len(ids) == n and all(ids[k] + 1 == ids[k + 1] for k in range(n - 1)):
                bank_state["i"] += n - 1
                return ids

    def psv(b, lo=0, hi=512):
        return psall[:, b * 512 + lo: b * 512 + hi]

    def psb(b, lo=0, hi=1024):
        return psall[:, b * 512: (b + 1) * 512].bitcast(BF16)[:, lo:hi]

    def R(b):
        return "ps%d" % b

    def mm(out, lhsT, rhs, start, stop, reads, writes, skip=False):
        if skip:
            P.op("pe", lambda e: e.matmul(out, lhsT, rhs, start=start, stop=stop, skip_group_check=True), reads=reads, writes=writes)
        else:
            P.op("pe", lambda e: e.matmul(out, lhsT, rhs, start=start, stop=stop), reads=reads, writes=writes)

    def tr(out, in_, ident, reads, writes):
        P.op("pe", lambda e: e.transpose(out, in_, ident), reads=reads, writes=writes)

    def act(out, in_, func, reads, writes, **kw):
        P.op("act", lambda e: e.activation(out=out, in_=in_, func=func, **kw), reads=reads, writes=writes)

    def tt(eng, out, in0, in1, op, reads, writes):
        P.op(eng, lambda e: e.tensor_tensor(out, in0, in1, op), reads=reads, writes=writes)

    def ts(eng, out, in0, s1, s2, op0, op1, reads, writes, accum_out=None):
        if accum_out is None:
            P.op(eng, lambda e: e.tensor_scalar(out, in0, s1, s2, op0, op1), reads=reads, writes=writes)
        else:
            P.op(eng, lambda e: e.tensor_scalar(out, in0, s1, s2, op0, op1, accum_out=accum_out), reads=reads, writes=writes)

    def cp(eng, out, in_, reads, writes):
        P.op(eng, lambda e: e.tensor_copy(out, in_), reads=reads, writes=writes)

    def ms(eng, out, val, writes):
        P.op(eng, lambda e: e.memset(out, val), writes=writes)

    def rsqrt_small(tag, out, in_, scale, reads):
        act(out, in_, AF.Sqrt, reads=list(reads) + ["eps"], writes=[tag], scale=scale, bias=eps_col[0:out.shape[0], :])
        P.op("dve", lambda e: e.reciprocal(out, out), reads=[tag], writes=[tag])

    pa = Alloc(0, 48 * 1024)
    ident_b = pa([128], BF16)
    i30k_b = pa([128], BF16)
    jmat_b = pa([128], BF16)
    ident_f = pa([128], F32)
    d1 = pa([128], F32)
    cm = pa([6], F32)
    hmask = pa([128], F32)
    ones_cb = pa([2], BF16)
    ones_row = pa([128], BF16, parts=1)
    u64 = pa([128], BF16, parts=1)
    penrow = pa([512], BF16, parts=1)
    dpen = pa([512], BF16, parts=1)
    eps_col = pa([1], F32)
    valid = pa([NPT], F32)
    oml_bc = pa([512], F32)
    recg_bc = pa([512], F32)
    attg_bc = pa([512], F32)
    qg_bc = pa([64], F32)
    kg_bc = pa([64], F32)
    ikg_bc = pa([64], F32)
    ikb_bc = pa([64], F32)
    iw_all = pa([NB, 8], F32)
    att_all = pa([NB, 512], BF16)
    small = pa([64], F32)
    sh1c = pa([8], F32)
    a1c = pa([8], F32)
    n1gc = pa([8], F32)
    PERS_END = pa.o

    ld = lambda name, dst, q="sp", **kw: P.dma(q, "c_" + name, dst, T[name], writes=[name], **kw)
    ld("ident_b", ident_b); ld("i30k_b", i30k_b); ld("jmat_b", jmat_b); ld("ident_f", ident_f)
    ld("d1", d1); ld("cm", cm); ld("hmask", hmask); ld("u64", u64); ld("penrow", penrow); ld("dpen", dpen)
    ld("valid", valid)
    ms("pool", ones_cb, 1.0, ["ones_cb"])
    ms("pool", ones_row, 1.0, ["ones_row"])
    ms("pool", eps_col, EPS, ["eps"])
    bcl = lambda name, dst, n: P.dma("sp", "c_" + name, dst, T[name].to_broadcast([128, n]), writes=[name])
    bcl("rec_out_g", recg_bc, 512); bcl("attn_out_g", attg_bc, 512)
    bcl("q_norm_g", qg_bc, 64); bcl("k_norm_g", kg_bc, 64); bcl("ikg", ikg_bc, 64); bcl("ikb", ikb_bc, 64)
    ts("dve", qg_bc, qg_bc, 0.125, None, ALU.mult, ALU.bypass, ["q_norm_g"], ["q_norm_g"])
    A0 = PERS_END
    al = Alloc(A0, ARENA)
    wA = al([8, DIN], BF16)
    crow = al([DIN + 24], BF16, parts=1)
    ikT = al([S], BF16)
    Sst = al([4, 128], F32)
    A_WORK = al.o
    st_al = Alloc(A_WORK, ARENA)
    wst = [st_al([DIN], F32), st_al([DIN], F32)]
    lbt = st_al([2, 512], F32)
    sc_col = st_al([8], F32)
    ccol = st_al([8], F32)
    wada = [st_al([8, 512], F32), st_al([8, 512], F32)]
    brow = st_al([512], F32, parts=1)
    mrow = [st_al([512], F32, parts=1), st_al([512], F32, parts=1)]
    sh1b = st_al([8], BF16)

    for k in range(8):
        wsb = wst[k % 2]
        P.dma("act", "wst%d" % (k % 2), wsb, T["w_in"][k * 128:(k + 1) * 128, :], writes=["wst%d" % (k % 2)])
        cp("dve" if k % 2 == 0 else "pool", wA[:, k, :], wsb, ["wst%d" % (k % 2)], ["wA"])

    P.dma("sp", "c_lb", lbt[:, 0, :], T["lb_logits"][0:1, :].to_broadcast([128, 512]), writes=["lb0"])
    P.dma("sp", "c_lb", lbt[:, 1, :], T["lb_logits"][1:2, :].to_broadcast([128, 512]), writes=["lb1"])
    tt("dve", lbt[:, 0, :], lbt[:, 1, :], lbt[:, 0, :], ALU.subtract, ["lb0", "lb1"], ["lb0"])
    act(oml_bc, lbt[:, 0, :], AF.Sigmoid, ["lb0"], ["oml"])

    ld("c_col", ccol)
    act(sc_col, ccol, AF.Silu, ["c_col"], ["sc_col"])
    wada_v = T["w_ada"].rearrange("(k p) c -> p k c", p=128)
    for g in range(12):
        wb = wada[g % 2]
        P.dma("sp", "wada%d" % (g % 2), wb, wada_v[:, :, g * 512:(g + 1) * 512], writes=["wada%d" % (g % 2)])
        P.dma("sp", "brow", brow, T["b_ada"][:, g * 512:(g + 1) * 512], writes=["brow"])
        b = bank()[0]
        for k in range(8):
            mm(psv(b)[0:1, :], sc_col[:, k:k + 1], wb[:, k, :], k == 0, k == 7,
               ["sc_col", "wada%d" % (g % 2)], [R(b)])
        mr = mrow[g % 2]
        tt("dve", mr, psv(b)[0:1, :], brow, ALU.add, [R(b), "brow"], ["mrow%d" % (g % 2)])
        P.dma("sp", "modw", mod_scr.ap()[:, g * 512:(g + 1) * 512], mr, reads=["mrow%d" % (g % 2)], writes=["mod_scr"])
    modv = mod_scr.ap()
    P.dma("sp", "c_sh1", sh1c, modv[0, 0:1024].rearrange("(k p) -> p k", p=128), reads=["mod_scr"], writes=["sh1c"],
          allow_slow_non_contiguous=True)
    P.dma("sp", "c_sc1", a1c, modv[0, 1024:2048].rearrange("(k p) -> p k", p=128), reads=["mod_scr"], writes=["a1c"],
          allow_slow_non_contiguous=True)
    ld("n1g_col", n1gc)
    ts("dve", a1c, a1c, 1.0, None, ALU.add, ALU.bypass, ["a1c"], ["a1c"])
    tt("dve", a1c, a1c, n1gc, ALU.mult, ["a1c", "n1g_col"], ["a1c"])

    cp("dve", sh1b, sh1c, ["sh1c"], ["sh1b"])
    for g in range(9):
        col0 = g * 512
        w_ = min(512, DIN - col0)
        b = bank()[0]
        for k in range(8):
            mm(psv(b, 0, w_)[0:1, :], sh1b[:, k:k + 1], wA[:, k, col0:col0 + w_], k == 0, k == 7, ["sh1b", "wA"], [R(b)])
        cp("dve", crow[:, col0:col0 + w_], psv(b, 0, w_)[0:1, :], [R(b)], ["crow"])
    ms("pool", Sst, 0.0, ["S"])

    rb_s = st_al([8], F32)
    oh_s = st_al([384], F32)
    fv_s = st_al([384], F32)
    P.dma("sp", "c_rb", rb_s[0:32, :], T["rel_bias"], writes=["rb"])
    P.dma("sp", "c_oh", oh_s[0:32, :], T["oh"], writes=["oh"])
    mm(psv(1)[0:8, 0:384], rb_s[0:32, :], oh_s[0:32, :], True, True, ["rb", "oh"], [R(1)])
    cp("dve", fv_s[0:8, :], psv(1)[0:8, 0:384], [R(1)], ["fv_s"])
    P.dma("sp", "fvw", fv_scr.ap(), fv_s[0:8, :], reads=["fv_s"], writes=["fv_scr"])

    wk = Alloc(A_WORK, ARENA)
    xt = [wk([8, TTW], F32), wk([8, TTW], F32)]
    xb = wk([8, TTW], BF16)
    xsq = wk([8, TTW], BF16)
    PB = lambda shape, dt, **kw: [wk(shape, dt, **kw), wk(shape, dt, **kw)]
    f_a = PB([512], F32); f_b = PB([512], F32); f_c = PB([512], F32); f_d = PB([512], F32); f_f = PB([512], F32)
    f_g = PB([512], BF16)
    ktil = PB([512], BF16); vbf = PB([512], BF16); knb = PB([512], BF16)
    vext = PB([8, 65], BF16)
    kTt = PB([4, 128], BF16)
    ikn2 = PB([128], BF16)
    ikr_ = PB([64], F32)
    ecols = PB([24], F32)
    colt = PB([32], F32)
    rmsrow = PB([128], BF16, parts=1)
    f_e = wk([512], F32)
    qtil = wk([512], BF16)
    tmp4 = wk([4, 128], F32)
    Sm = [wk([4, 128], BF16), wk([4, 128], BF16)]
    kq_T = wk([8, 128], BF16)
    A_T = wk([4, 128], BF16)
    recraw = wk([512], F32)
    qTt = wk([4, 128], BF16)
    recb = wk([512], BF16)
    iqTt = wk([4, 128], BF16)
    A_END = wk.o

    P.barrier()
    ms("pool", vext[0], 1.0, ["vext0"])
    ms("pool", vext[1], 1.0, ["vext1"])

    xT_v = T["xT"].rearrange("(k p) t -> p k t", p=128)

    def proj(q, col0, width, s_):
        b = bank()[0]
        for k in range(8):
            mm(psv(b, 0, width), xb[:, k, q * 128:(q + 1) * 128], wA[:, k, col0:col0 + width], k == 0, False,
               ["xb", "wA"], [R(b)])
        mm(psv(b, 0, width), rmsrow[s_][0:1, :], crow[0:1, col0:col0 + width], False, True, ["rmsrow%d" % s_, "crow"], [R(b)])
        return b

    def headnorm(src, nh, hd, gbc, dst, tagp, gtag, s_, dtag=None, eng2="pool"):
        sq = f_g[s_]
        sqn = "f_g%d" % s_
        cn = "colt_ss%d" % s_
        act(sq[:, 0:nh * hd], src, AF.Square, [tagp], [sqn])
        ss = colt[s_][:, 0:nh]
        P.op("dve", lambda e: e.tensor_reduce(ss, sq[:, 0:nh * hd].rearrange("p (h d) -> p h d", h=nh), AX.X, ALU.add),
             reads=[sqn], writes=[cn])
        rsqrt_small(cn, ss, ss, 1.0 / hd, [cn])
        s3 = src.rearrange("p (h d) -> p h d", h=nh)
        tt("dve", s3, s3, ss.unsqueeze(2).to_broadcast([128, nh, hd]), ALU.mult, [tagp, cn], [tagp])
        g3 = gbc.unsqueeze(1).to_broadcast([128, nh, hd]) if gbc.shape[1] == hd else gbc.rearrange("p (h d) -> p h d", h=nh)
        tt(eng2, dst.rearrange("p (h d) -> p h d", h=nh), s3, g3, ALU.mult, [tagp, gtag], [dtag or tagp])

    f_q = wk([512], F32); f_gate = wk([512], F32); f_aq = wk([512], F32)
    iqb = wk([512], BF16)

    def xload(t_):
        P.dma("sp", "xt%d" % (t_ % 2), xt[t_ % 2], xT_v[:, :, t_ * TTW:(t_ + 1) * TTW], writes=["xt%d" % (t_ % 2)])

    def xcast(tti):
        xtb = xt[tti % 2]
        xr = "xt%d" % (tti % 2)
        tt("dve", xb, xtb, a1c.unsqueeze(2).to_broadcast([128, 8, TTW]), ALU.mult, [xr, "a1c"], ["xb"])
        act(xsq, xtb, AF.Square, [xr], ["xsq"])
        if tti + 1 < NTT:
            xload(tti + 1)

    def early(p):
        q = p % (TTW // 128)
        own = (p % 4 == 3)
        i = p // 4
        s_ = p % 2
        N = lambda base: base + str(s_)
        b = bank()[0]
        for k in range(8):
            mm(psv(b, 0, 1), xsq[:, k, q * 128:(q + 1) * 128], ones_cb[:, 0:1], k == 0, k == 7, ["xsq", "ones_cb"], [R(b)])
        for k in range(8):
            mm(psv(b, 128, 256)[0:1, :], ones_cb[:, 0:1], xsq[:, k, q * 128:(q + 1) * 128], k == 0, k == 7, ["xsq", "ones_cb"], [R(b)])
        act(rmsrow[s_], psv(b, 128, 256)[0:1, :], AF.Sqrt, [R(b), "eps"], [N("rmsrow")], scale=1.0 / D, bias=eps_col[0:1, :])
        ct = colt[s_]
        rstd = ct[:, 16:17]; nrstd = ct[:, 17:18]; rstd8 = ct[:, 18:19]
        rs = N("rstd")
        act(rstd, psv(b, 0, 1), AF.Sqrt, [R(b), "eps"], [rs], scale=1.0 / D, bias=eps_col)
        P.op("dve", lambda e, o=rstd: e.reciprocal(o, o), reads=[rs], writes=[rs])
        tt("dve", rstd, rstd, valid[:, p:p + 1], ALU.mult, [rs, "valid"], [rs])
        ts("dve", nrstd, rstd, -1.0, None, ALU.mult, ALU.bypass, [rs], [N("nrstd")])
        b_rf = proj(q, 512, 512, s_)
        act(f_a[s_], psv(b_rf), AF.Sigmoid, [R(b_rf), N("nrstd")], [N("f_a")], scale=nrstd)
        tt("dve", f_b[s_], f_a[s_], oml_bc, ALU.mult, [N("f_a"), "oml"], [N("f_b")])
        act(f_c[s_], f_b[s_], AF.Ln, [N("f_b")], [N("f_c")], scale=-1.0, bias=1.0)
        b_ri = proj(q, 1024, 512, s_)
        ts("dve", vbf[s_], psv(b_ri), rstd, None, ALU.mult, ALU.bypass, [R(b_ri), rs], [N("vbf")])
        b_ak = proj(q, 2560, 512, s_)
        ts("dve", f_f[s_], psv(b_ak), rstd, None, ALU.mult, ALU.bypass, [R(b_ak), rs], [N("f_f")])
        b_av = proj(q, 3072, 512, s_)
        ve = vext[s_]
        ts("dve", ve[:, :, 0:64], psv(b_av).rearrange("p (h d) -> p h d", h=8), rstd, None, ALU.mult, ALU.bypass,
           [R(b_av), rs], [N("vext")])
        P.dma("sp", N("vw"), v_scr.ap()[p * 128:(p + 1) * 128, :].rearrange("s (h d) -> s h d", h=8), ve,
              reads=[N("vext")], writes=["v_scr"])
        b_ik = proj(q, 4096, 64, s_)
        ts("dve", ikr_[s_], psv(b_ik, 0, 64), rstd, None, ALU.mult, ALU.bypass, [R(b_ik), rs], [N("ikr")])
        if own:
            b_rq = proj(q, 0, 512, s_)
            act(f_q, psv(b_rq), AF.Silu, [R(b_rq), rs], ["f_q"], scale=rstd)
            b_rg = proj(q, 1536, 512, s_)
            act(f_gate, psv(b_rg), AF.Silu, [R(b_rg), rs], ["f_gate"], scale=rstd)
            b_aq = proj(q, 2048, 512, s_)
            ts("dve", f_aq, psv(b_aq), rstd, None, ALU.mult, ALU.bypass, [R(b_aq), rs], ["f_aq"])
            b_iq = proj(q, 3584, 512, s_)
            ts("dve", rstd8, rstd, 0.125, None, ALU.mult, ALU.bypass, [rs], [N("rstd8")])
            ts("dve", iqb, psv(b_iq), rstd8, None, ALU.mult, ALU.bypass, [R(b_iq), N("rstd8")], ["iqb"])
            b_iw = proj(q, 4160, 8, s_)
            ts("dve", rstd8, rstd, float(8 ** -0.5), None, ALU.mult, ALU.bypass, [rs, N("rstd8")], [N("rstd8")])
            ts("dve", iw_all[:, i, :], psv(b_iw, 0, 8), rstd8, None, ALU.mult, ALU.bypass, [R(b_iw), N("rstd8")], ["iw%d" % i])

    def late(p):
        own = (p % 4 == 3)
        i = p // 4
        s_ = p % 2
        N = lambda base: base + str(s_)
        ct = colt[s_]
        ikr = ikr_[s_]
        b_b = bank()[0]
        mm(psv(b_b), d1, f_c[s_], True, True, ["d1", N("f_c")], [R(b_b)])
        b_c = bank()[0]
        for h in range(4):
            mm(psv(b_c, h * 6, h * 6 + 6), f_c[s_][:, h * 128:(h + 1) * 128], cm, True, True, [N("f_c"), "cm"], [R(b_c)])
        act(f_d[s_], psv(b_b), AF.Exp, [R(b_b)], [N("f_d")], scale=-1.0)
        tt("pool", ktil[s_], f_b[s_], f_d[s_], ALU.mult, [N("f_b"), N("f_d")], [N("ktil")])
        act(ecols[s_], psv(b_c, 0, 24), AF.Exp, [R(b_c)], [N("ecols")])
        if own:
            act(f_e, psv(b_b), AF.Exp, [R(b_b)], ["f_e"])
        headnorm(f_f[s_], 8, 64, kg_bc, knb[s_], N("f_f"), "k_norm_g", s_, dtag=N("knb"))
        st6 = ct[:, 20:26]; mv = ct[:, 26:28]; irs = ct[:, 28:29]
        P.op("dve", lambda e, o=st6, i_=ikr: e.bn_stats(o, i_), reads=[N("ikr")], writes=[N("st6")])
        P.op("dve", lambda e, o=mv, i_=st6: e.bn_aggr(o, i_), reads=[N("st6")], writes=[N("mv")])
        rsqrt_small(N("ikrs"), irs, mv[:, 1:2], 1.0, [N("mv")])
        ts("dve", ikr, ikr, mv[:, 0:1], irs, ALU.subtract, ALU.mult, [N("ikr"), N("mv"), N("ikrs")], [N("ikr")])
        tt("dve", ikr, ikr, ikg_bc, ALU.mult, [N("ikr"), "ikg"], [N("ikr")])
        tt("dve", ikn2[s_][:, 0:64], ikr, ikb_bc, ALU.add, [N("ikr"), "ikb"], [N("ikn2")])
        cp("pool", ikn2[s_][:, 64:128], ikn2[s_][:, 0:64], [N("ikn2")], [N("ikn2")])

    def late2(p):
        own = (p % 4 == 3)
        i = p // 4
        s_ = p % 2
        N = lambda base: base + str(s_)
        bu = bank(2)
        for c in range(2):
            for h in range(4):
                mm(psv(bu[c], h * 128, (h + 1) * 128), ktil[s_][64 * c:64 * c + 64, h * 128:(h + 1) * 128],
                   vbf[s_][64 * c:64 * c + 64, h * 128:(h + 1) * 128], True, True, [N("ktil"), N("vbf")], [R(bu[c])])
        bt = bank()[0]
        for hp in range(4):
            tr(psb(bt, hp * 128, (hp + 1) * 128), knb[s_][:, hp * 128:(hp + 1) * 128], ident_b, [N("knb"), "ident_b"], [R(bt)])
        kt_ = kTt[s_]
        act(kt_, psb(bt, 0, 512).rearrange("p (a b) -> p a b", a=4), AF.Copy, [R(bt)], [N("kTt")])
        P.dma("sp", N("kTw"), kT_scr.ap().rearrange("(a r) s -> r a s", a=4)[:, :, p * 128:(p + 1) * 128], kt_,
              reads=[N("kTt")], writes=["kT_scr"])
        bt2 = bank()[0]
        tr(psb(bt2, 0, 128), ikn2[s_], ident_b, [N("ikn2"), "ident_b"], [R(bt2)])
        act(ikT[:, p * 128:(p + 1) * 128], psb(bt2, 0, 128), AF.Copy, [R(bt2)], ["ikT"])
        e3 = ecols[s_].rearrange("p (h x) -> p h x", h=4)
        for c in range(2):
            if own:
                tt("dve", Sm[c], Sst, e3[:, :, 3 * c:3 * c + 1].to_broadcast([128, 4, 128]), ALU.mult,
                   ["S", N("ecols")], ["Sm%d" % c])
            tt("dve", tmp4, psv(bu[c]).rearrange("p (h x) -> p h x", h=4),
               e3[:, :, 3 * c + 2:3 * c + 3].to_broadcast([128, 4, 128]), ALU.mult, [R(bu[c]), N("ecols")], ["tmp4"])
            tt("pool", Sst, Sst, e3[:, :, 3 * c + 1:3 * c + 2].to_broadcast([128, 4, 128]), ALU.mult, ["S", N("ecols")], ["S"])
            tt("pool", Sst, Sst, tmp4, ALU.add, ["S", "tmp4"], ["S"])
        if not own:
            return
        tt("pool", qtil, f_q, f_e, ALU.mult, ["f_q", "f_e"], ["qtil"])
        bt3 = bank()[0]
        for h in range(4):
            tr(psb(bt3, h * 128, (h + 1) * 128), ktil[s_][:, h * 128:(h + 1) * 128], ident_b, [N("ktil"), "ident_b"], [R(bt3)])
        act(kq_T[:, 0:4, :], psb(bt3, 0, 512).rearrange("p (a b) -> p a b", a=4), AF.Copy, [R(bt3)], ["kq_Ta"])
        bt4 = bank()[0]
        for h in range(4):
            tr(psb(bt4, h * 128, (h + 1) * 128), qtil[:, h * 128:(h + 1) * 128], ident_b, ["qtil", "ident_b"], [R(bt4)])
        P.op("dve", lambda e, o=kq_T[:, 4:8, :], i_=psb(bt4, 0, 512).rearrange("p (a b) -> p a b", a=4): e.tensor_copy(o, i_),
             reads=[R(bt4)], writes=["kq_Tb"])
        bt6 = bank()[0]
        for hp in range(4):
            tr(psb(bt6, hp * 128, (hp + 1) * 128), iqb[:, hp * 128:(hp + 1) * 128], ident_b, ["iqb", "ident_b"], [R(bt6)])
        act(iqTt, psb(bt6, 0, 512).rearrange("p (a b) -> p a b", a=4), AF.Copy, [R(bt6)], ["iqTt"])
        P.dma("sp", "iqTw", iqT_scr.ap()[i * 128:(i + 1) * 128, :].rearrange("p (a t) -> p a t", a=4), iqTt,
              reads=["iqTt"], writes=["iqT_scr"])
        ba = bank()[0]
        for h in range(4):
            mm(psv(ba, h * 128, (h + 1) * 128), kq_T[:, h, :], kq_T[:, 4 + h, :], True, True, ["kq_Ta", "kq_Tb"], [R(ba)])
        tt("dve", A_T, psv(ba).rearrange("p (h x) -> p h x", h=4), hmask.unsqueeze(1).to_broadcast([128, 4, 128]),
           ALU.mult, [R(ba), "hmask"], ["A_T"])
        headnorm(f_aq, 8, 64, qg_bc, qtil, "f_aq", "q_norm_g", s_, dtag="qtil")
        bo = bank(2)
        for c in range(2):
            for h in range(4):
                mm(psv(bo[c], h * 128, (h + 1) * 128), A_T[:, h, :], vbf[s_][:, h * 128:(h + 1) * 128], True, False,
                   ["A_T", N("vbf")], [R(bo[c])])
                mm(psv(bo[c], h * 128, (h + 1) * 128), kq_T[:, 4 + h, :], Sm[c][:, h, :], False, True,
                   ["kq_Tb", "Sm%d" % c], [R(bo[c])])
        cp("dve", recraw[0:64, :], psv(bo[0])[0:64, :], [R(bo[0])], ["recraw"])
        cp("dve", recraw[64:128, :], psv(bo[1])[64:128, :], [R(bo[1])], ["recraw"])
        bt5 = bank()[0]
        for hp in range(4):
            tr(psb(bt5, hp * 128, (hp + 1) * 128), qtil[:, hp * 128:(hp + 1) * 128], ident_b, ["qtil", "ident_b"], [R(bt5)])
        act(qTt, psb(bt5, 0, 512).rearrange("p (a b) -> p a b", a=4), AF.Copy, [R(bt5)], ["qTt"])
        P.dma("sp", "qTw", qT_scr.ap()[i * 128:(i + 1) * 128, :].rearrange("p (a t) -> p a t", a=4), qTt,
              reads=["qTt"], writes=["qT_scr"])
        headnorm(recraw, 4, 128, recg_bc, recraw, "recraw", "rec_out_g", s_)
        tt("pool", recb, recraw, f_gate, ALU.mult, ["recraw", "f_gate"], ["recb"])
        P.dma("sp", "recw", rec_scr.ap()[i * 128:(i + 1) * 128, :], recb, reads=["recb"], writes=["rec_scr"])

    TPT = TTW // 128
    xload(0)
    xcast(0)
    early(0)
    late(0)
    for p in range(NPT):
        if p + 1 < NPT:
            if (p + 1) % TPT == 0:
                xcast((p + 1) // TPT)
            early(p + 1)
        late2(p)
        if p + 1 < NPT:
            late(p + 1)

    P.barrier()
    al2 = Alloc(A0, ARENA)
    _wA = al2([8, DIN], BF16); _cr = al2([DIN + 24], BF16, parts=1)
    IKT_OFF = al2.o
    _ik = al2([S], BF16)
    IKT_END = al2.o
    r1 = Alloc(A0, IKT_OFF)
    r2 = Alloc(IKT_END, ARENA)
    NVT = 4
    score = [r1([S], F32), r1([S], F32)]
    kbuf = [r1([4, NVT * 128], BF16), r1([4, NVT * 128], BF16)]
    mneg = [r2([S], BF16), r2([S], BF16)]
    vbuf = [r2([NVT, 520], BF16), r2([NVT, 520], BF16)]
    relu_b = [r2([512], BF16), r2([512], BF16), r2([512], BF16)]
    diagw = r2([8, 128], BF16)
    hank = r2([2, 8, 128], BF16)
    i30k4 = r2([512], BF16)
    iqz = [r2([8, 128], BF16), r2([8, 128], BF16)]
    qbd = [r2([4, 256], BF16), r2([4, 256], BF16)]
    pT = [r2([4, 128], BF16), r2([4, 128], BF16), r2([4, 128], BF16)]
    attraw = r2([512], F32)
    bis = [r2([32], F32), r2([32], F32)]
    hank_f = score[0][:, 0:2048].rearrange("p (a b) -> p a b", a=16)

    P.dma("sp", "c_i30k4", i30k4, T["i30k4_b"], writes=["i30k4"])
    for dl in range(2):
        for h in range(8):
            P.dma("sp", "hkl", hank_f[:, dl * 8 + h, :], bass.AP(fv_scr, h * 384 + (128 if dl == 1 else 0), [[1, 128], [1, 128]]),
                  reads=["fv_scr"], writes=["score0"])
    cp("dve", hank.rearrange("p a b c -> p (a b) c"), hank_f, ["score0"], ["hank"])
    ms("pool", qbd[0], 0.0, ["qbd0"])
    ms("pool", qbd[1], 0.0, ["qbd1"])
    ms("pool", iqz[0], 0.0, ["iqz0"])
    ms("pool", iqz[1], 0.0, ["iqz1"])

    OB = [6, 7]
    v_v = v_scr.ap().rearrange("(t s) c -> s t c", s=128)
    kT_v = kT_scr.ap().rearrange("(a r) s -> r a s", a=4)
    vchunks = [(i_, c_) for i_ in range(NB) for c_ in range((4 * (i_ + 1)) // NVT)]
    gcbase = [0]
    for i_ in range(NB):
        gcbase.append(gcbase[-1] + (4 * (i_ + 1)) // NVT)

    def vload(gc):
        i_, c_ = vchunks[gc]
        P.dma("sp", "vbuf%d" % (gc % 2), vbuf[gc % 2], v_v[:, c_ * NVT:(c_ + 1) * NVT, :], reads=["v_scr"], writes=["vbuf%d" % (gc % 2)])
        P.dma("sp", "kbuf%d" % (gc % 2), kbuf[gc % 2], kT_v[:, :, c_ * NVT * 128:(c_ + 1) * NVT * 128], reads=["kT_scr"], writes=["kbuf%d" % (gc % 2)])

    def indexer(i):
        G = i + 1
        par = i % 2
        iqn = "iqz%d" % par
        sc_ = score[par]
        scn = "score%d" % par
        iqv = iqT_scr.ap()[i * 128:(i + 1) * 128, :].rearrange("p (a t) -> p a t", a=4)
        iz4 = iqz[par].rearrange("p (a two) t -> p a two t", two=2)
        P.dma("sp", "iqTl%d" % par, iz4[0:64, :, 0, :], iqv[0:64], reads=["iqT_scr"], writes=[iqn])
        P.dma("sp", "iqTl%d" % par, iz4[64:128, :, 1, :], iqv[64:128], reads=["iqT_scr"], writes=[iqn])
        for h in range(8):
            ts("pool", diagw[:, h, :], ident_b, iw_all[:, i, h:h + 1], None, ALU.mult, ALU.bypass, ["ident_b", "iw%d" % i], ["diagw"])

        def qk_relu(g, h):
            bs = (g * 8 + h) % 4
            mm(psv(bs), iqz[par][:, h, :], ikT[:, g * 512:(g + 1) * 512], True, True, [iqn, "ikT"], [R(bs)])
            k3 = (g * 8 + h) % 3
            act(relu_b[k3], psv(bs), AF.Relu, [R(bs)], ["relu%d" % k3])

        def accum(g, h):
            bacc = 4 + (g % 2)
            k3 = (g * 8 + h) % 3
            rb = relu_b[k3]
            rn = "relu%d" % k3
            last = (h == 7) and (g != 0) and (g != G - 1)
            mm(psv(bacc), diagw[:, h, :], rb, h == 0, last, ["diagw", rn], [R(bacc)])
            if h == 7:
                if g == 0:
                    mm(psv(bacc), ones_row[0:1, :], dpen[0:1, :], False, g != G - 1, ["ones_row", "dpen"], [R(bacc)])
                if g == G - 1:
                    mm(psv(bacc), u64[0:1, :], penrow[0:1, :], False, True, ["u64", "penrow"], [R(bacc)])
                act(sc_[:, g * 512:(g + 1) * 512], psv(bacc), AF.Relu, [R(bacc), "hank"], [scn], bias=64.0)

        prev = None
        for g in range(G):
            for h in range(8):
                qk_relu(g, h)
                if prev is not None:
                    accum(*prev)
                prev = (g, h)
        accum(*prev)

    def bisect(i):
        n = 512 * (i + 1)
        par = i % 2
        b_ = bis[par]
        sc_ = score[par]
        scn = "score%d" % par
        mid = b_[:, 1:2]; cnt = b_[:, 2:3]; tmpc = b_[:, 3:4]; lo = b_[:, 0:1]
        mn = mneg[par]
        mname = "mneg%d" % par
        bn = "bis%d" % par
        ms("dve", mid, 1.0 + 127.0 / 2, [bn])
        for it in range(NBIS):
            wk_ = 127.0 / (2 ** (it + 1))
            ts("dve", mn[:, 0:n], sc_[:, 0:n], mid, None, ALU.is_ge, ALU.add, [scn, bn], [mname, bn], accum_out=cnt)
            ts("dve", tmpc, cnt, 255.5, wk_, ALU.is_ge, ALU.mult, [bn], [bn])
            P.op("dve", lambda e, w=wk_, o=mid, t_=tmpc: e.scalar_tensor_tensor(o, t_, -w / 2, o, ALU.add, ALU.add), reads=[bn], writes=[bn])
        ts("dve", lo, mid, -127.0 / (2 ** (NBIS + 1)), None, ALU.add, ALU.bypass, [bn], [bn])
        ts("dve", mn[:, 0:n], sc_[:, 0:n], lo, 1.0, ALU.is_ge, ALU.subtract, [scn, bn], [mname])

    def attention(i):
        NKT = 4 * (i + 1)
        par = i % 2
        mn = mneg[par]
        mname = "mneg%d" % par
        qn = "qbd%d" % par
        qv = qT_scr.ap()[i * 128:(i + 1) * 128, :].rearrange("p (a t) -> p a t", a=4)
        P.dma("sp", "qbdl%d" % par, qbd[par][0:64, :, 0:128], qv[0:64], reads=["qT_scr"], writes=[qn])
        P.dma("sp", "qbdl%d" % par, qbd[par][64:128, :, 128:256], qv[64:128], reads=["qT_scr"], writes=[qn])
        steps = [(kt, hh) for kt in range(NKT) for hh in range(2)]
        info = {}

        def qk(kt, hh):
            near = kt >= NKT - 2
            dl = 0 if kt == NKT - 2 else 1
            gc = gcbase[i] + kt // NVT
            kb = kbuf[gc % 2]
            kr = "kbuf%d" % (gc % 2)
            bL = bank()[0]
            mm(psv(bL), mn[:, kt * 128:(kt + 1) * 128], i30k4, True, False, [mname, "i30k4"], [R(bL)])
            for pp in range(2):
                hp = hh * 2 + pp
                mm(psv(bL, pp * 256, (pp + 1) * 256), kb[:, hp, (kt % NVT) * 128:(kt % NVT + 1) * 128], qbd[par][:, hp, :], False,
                   (not near) and pp == 1, [kr, qn], [R(bL)])
            if near:
                for h4 in range(4):
                    h = hh * 4 + h4
                    mm(psv(bL, h4 * 128, (h4 + 1) * 128), hank[:, dl, h, :], jmat_b, False, h4 == 3, ["hank", "jmat_b"], [R(bL)])
            k3 = (kt * 2 + hh) % 3
            pt = pT[k3]
            pr = "pT%d" % k3
            act(pt, psv(bL).rearrange("p (a b) -> p a b", a=4), AF.Exp, [R(bL)], [pr])
            info[(kt, hh)] = (pt, pr)

        def pv(kt, hh):
            gc = gcbase[i] + kt // NVT
            if kt % NVT == 0 and hh == 0 and gc + 1 < len(vchunks):
                vload(gc + 1)
            vb = vbuf[gc % 2]
            vr = "vbuf%d" % (gc % 2)
            pt, pr = info[(kt, hh)]
            for h4 in range(4):
                h = hh * 4 + h4
                mm(psv(OB[hh], h4 * 128, h4 * 128 + 65), pt[:, h4, :], vb[:, kt % NVT, h * 65:(h + 1) * 65],
                   kt == 0 and h4 == 0, kt == NKT - 1, [pr, vr], [R(OB[hh])], skip=True)

        qk(*steps[0])
        for si in range(len(steps)):
            if si + 1 < len(steps):
                qk(*steps[si + 1])
            pv(*steps[si])

    def attnorm(i):
        b_ = bis[i % 2]
        den = b_[:, 4:12]
        for hh in range(2):
            o3 = psv(OB[hh]).rearrange("p (a b) -> p a b", a=4)
            P.op("dve", lambda e, o=den[:, hh * 4:hh * 4 + 4], i_=o3[:, :, 64]: e.reciprocal(o, i_), reads=[R(OB[hh])], writes=["den%d%d" % (i % 2, hh)])
            tt("dve", attraw[:, hh * 256:(hh + 1) * 256].rearrange("p (a b) -> p a b", a=4), o3[:, :, 0:64],
               den[:, hh * 4:hh * 4 + 4].unsqueeze(2).to_broadcast([128, 4, 64]), ALU.mult, [R(OB[hh]), "den%d%d" % (i % 2, hh)], ["attraw"])
        sqb = pT[0].rearrange("p a b -> p (a b)")
        act(sqb, attraw, AF.Square, ["attraw"], ["pT0"])
        ss8 = b_[:, 12:20]
        sn = "ss8%d" % (i % 2)
        P.op("dve", lambda e, o=ss8, i_=sqb.rearrange("p (h d) -> p h d", h=8): e.tensor_reduce(o, i_, AX.X, ALU.add), reads=["pT0"], writes=[sn])
        rsqrt_small(sn, ss8, ss8, 1.0 / 64, [sn])
        a3 = attraw.rearrange("p (h d) -> p h d", h=8)
        tt("dve", a3, a3, ss8.unsqueeze(2).to_broadcast([128, 8, 64]), ALU.mult, ["attraw", sn], ["attraw"])
        tt("pool", att_all[:, i, :], attraw, attg_bc, ALU.mult, ["attraw", "attn_out_g"], ["att_all%d" % i])

    bank_state["set"] = list(range(4))
    bank_state["i"] = 0
    vload(0)
    indexer(0)
    bisect(0)
    indexer(1)
    for i in range(NB):
        if i + 2 < NB:
            indexer(i + 2)
        attention(i)
        if i + 1 < NB:
            bisect(i + 1)
        attnorm(i)

    if DEBUG:
        P.barrier()
        dacc = view(A0, [NB, D], F32)
        for i in range(NB):
            P.dma("sp", "recl", dacc[:, i, 0:256].bitcast(BF16) if False else view(A0 + 80 * 1024, [512], BF16), rec_scr.ap()[i * 128:(i + 1) * 128, :], reads=["rec_scr"], writes=["recd"])
            cp("dve", dacc[:, i, 0:512], view(A0 + 80 * 1024, [512], BF16), ["recd"], ["dacc%d" % i])
            cp("dve", dacc[:, i, 512:1024], att_all[:, i, :], ["att_all%d" % i], ["dacc%d" % i])
            P.dma("sp", "outw", out_d.rearrange("(i p) d -> p i d", p=128)[:, i, :], dacc[:, i, :], reads=["dacc%d" % i], writes=["out"])
        P.final_wait("sp", ["out"])
        P.emit()
        return nc

    PHB = ["kT_res", "score", "mneg", "vbuf0", "vbuf1", "relu0", "relu1", "relu2", "diagw", "hank", "qTb", "iqTb",
           "pT0", "pT1", "pT2", "attraw0", "attraw1", "ikT", "ss8", "rs8", "lo", "mid", "cnt", "ge", "den0", "den1"]
    P.barrier()
    bank_state["set"] = list(range(8))
    bank_state["i"] = 0
    comb_scr = nc.dram_tensor("comb_scr", [16, NB * 128], F32, kind="Internal")
    cl = Alloc(A0, ARENA)
    acc = cl([NB, D], F32)
    h2T = cl([8, NB * 128], BF16)
    WX0 = cl.o
    wexp = [[cl([8, 512], BF16), cl([8, 512], BF16), cl([4, D], BF16)] for _ in range(2)]
    WX1 = cl.o
    stg = [cl([1024], F32), cl([1024], F32)]
    wr_b = cl([8, 20], BF16)
    wr_f = cl([8, 20], F32)
    brbc = cl([20], F32)
    rt2 = [cl([128], F32), cl([128], F32)]
    cstage = cl([128], F32)
    recc = [cl([512], BF16), cl([512], BF16)]
    he = [cl([4, 512], BF16), cl([4, 512], BF16)]
    cbt = [cl([512], F32), cl([512], F32)]
    s1t = [cl([512], BF16), cl([512], BF16)]
    t3t = [cl([512], BF16), cl([512], BF16)]
    g2bc = cl([D], F32)
    C_END = cl.o
    c1 = Alloc(WX0, WX1)
    wout_b = c1([8, D], BF16)
    g1bc = c1([D], F32); a2bc = c1([D], F32); sh2bc = c1([D], F32)
    mixT = [c1([8, 128], BF16), c1([8, 128], BF16)]
    tmpf = [c1([D], F32), c1([D], F32)]
    h2b = [c1([D], BF16), c1([D], BF16)]
    C1RES = ["wout_b", "g1bc", "a2bc", "sh2bc", "mixT", "tmpf", "h2b"]

    first = PHB
    modb = lambda dst, k, name: P.dma("sp", "c_" + name, dst, modv[:, k * D:(k + 1) * D].to_broadcast([128, D]),
                                      reads=["mod_scr"], writes=[name] + first)
    modb(g1bc, 2, "g1bc"); modb(sh2bc, 3, "sh2bc"); modb(a2bc, 4, "a2bc")
    P.dma("sp", "c_n2g", tmpf[0], T["n2g"].to_broadcast([128, D]), writes=["tmpf0"] + first)
    ts("dve", a2bc, a2bc, 1.0, None, ALU.add, ALU.bypass, ["a2bc"], ["a2bc"])
    tt("dve", a2bc, a2bc, tmpf[0], ALU.mult, ["a2bc", "tmpf0"], ["a2bc"])
    P.dma("sp", "c_wr", wr_f, T["w_r"].rearrange("(k p) c -> p k c", p=128), writes=["wr_f"] + first)
    cp("dve", wr_b, wr_f, ["wr_f"], ["wr_b"] + first)
    P.dma("sp", "c_br", brbc, T["b_r"].to_broadcast([128, 20]), writes=["brbc"] + first)
    wo_v = T["w_out"].rearrange("(k p) c -> p k c", p=128)
    for k in range(8):
        sb = stg[k % 2]
        P.dma("sp", "stg%d" % (k % 2), sb, wo_v[:, k, :], writes=["stg%d" % (k % 2)] + (first if k < 2 else []))
        cp("pool" if k % 2 == 0 else "dve", wout_b[:, k, :], sb, ["stg%d" % (k % 2)], ["wout_b"] + (first if k == 0 else []))
    xo_v = T["x_own"].rearrange("(i p) d -> p i d", p=128)
    for i in range(NB):
        P.dma("sp", "xo", acc[:, i, :], xo_v[:, i, :], writes=["acc%d" % i] + (first if i == 0 else []))

    def recload(i_):
        P.dma("sp", "recc%d" % (i_ % 2), recc[i_ % 2], rec_scr.ap()[i_ * 128:(i_ + 1) * 128, :], reads=["rec_scr"], writes=["recc%d" % (i_ % 2)])

    def c1_main(i):
        s_ = i % 2
        N = lambda b_: b_ + str(s_)
        tf = tmpf[s_]
        if i + 1 < NB:
            recload(i + 1)
        bt = bank()[0]
        for k in range(8):
            src = recc[s_][:, k * 128:(k + 1) * 128] if k < 4 else att_all[:, i, (k - 4) * 128:(k - 3) * 128]
            tr(psb(bt, k * 128, (k + 1) * 128), src, ident_b, [N("recc"), "att_all%d" % i, "ident_b"], [R(bt)])
        act(mixT[s_], psb(bt).rearrange("p (a b) -> p a b", a=8), AF.Copy, [R(bt)], [N("mixT")])
        for half in range(2):
            b = bank()[0]
            for k in range(8):
                mm(psv(b), mixT[s_][:, k, :], wout_b[:, k, half * 512:(half + 1) * 512], k == 0, k == 7, [N("mixT"), "wout_b"], [R(b)])
            tt("dve", tf[:, half * 512:(half + 1) * 512], psv(b), g1bc[:, half * 512:(half + 1) * 512], ALU.mult, [R(b), "g1bc", "a2bc"], [N("tmpf")])
        tt("dve", acc[:, i, :], acc[:, i, :], tf, ALU.add, ["acc%d" % i, N("tmpf")], ["acc%d" % i])
        rt = rt2[s_]
        ss = rt[:, 0:1]
        act(tf, acc[:, i, :], AF.Square, ["acc%d" % i], [N("tmpf"), N("ss2")], accum_out=ss)
        rsqrt_small(N("ss2"), ss, ss, 1.0 / D, [N("ss2")])
        act(tf, acc[:, i, :], AF.Identity, ["acc%d" % i, N("ss2")], [N("tmpf")], scale=ss)
        tt("dve", tf, tf, a2bc, ALU.mult, [N("tmpf"), "a2bc"], [N("tmpf")])
        tt("dve", h2b[s_], tf, sh2bc, ALU.add, [N("tmpf"), "sh2bc"], [N("h2b")])
        bt = bank()[0]
        for k in range(8):
            tr(psb(bt, k * 128, (k + 1) * 128), h2b[s_][:, k * 128:(k + 1) * 128], ident_b, [N("h2b"), "ident_b"], [R(bt)])
        act(h2T[:, :, i * 128:(i + 1) * 128], psb(bt).rearrange("p (a b) -> p a b", a=8), AF.Copy, [R(bt)], ["h2T%d" % i])
        b = bank()[0]
        for k in range(8):
            mm(psv(b, 0, 20), h2T[:, k, i * 128:(i + 1) * 128], wr_b[:, k, :], k == 0, k == 7, ["h2T%d" % i, "wr_b"], [R(b)])
        lg = rt[:, 4:24]
        tt("dve", lg, psv(b, 0, 20), brbc, ALU.add, [R(b), "brbc"], [N("lg")])

    def c1_router(i):
        s_ = i % 2
        N = lambda b_: b_ + str(s_)
        rt = rt2[s_]
        lg = rt[:, 4:24]
        gmax = rt[:, 24:25]; ngmax = rt[:, 25:26]; sumg = rt[:, 26:27]; eg = rt[:, 28:32]
        P.op("dve", lambda e, o=gmax, i_=lg[:, 0:4]: e.tensor_reduce(o, i_, AX.X, ALU.max), reads=[N("lg")], writes=[N("gmax")])
        ts("dve", ngmax, gmax, -1.0, None, ALU.mult, ALU.bypass, [N("gmax")], [N("ngmax")])
        act(eg, lg[:, 0:4], AF.Exp, [N("lg"), N("ngmax")], [N("eg"), N("sumg")], bias=ngmax, accum_out=sumg)
        gate = rt[:, 27:28]
        P.op("dve", lambda e, o=gate, i_=sumg: e.reciprocal(o, i_), reads=[N("sumg")], writes=[N("gate")])
        ohg = rt[:, 32:36]
        ts("dve", ohg, lg[:, 0:4], gmax, None, ALU.is_ge, ALU.bypass, [N("lg"), N("gmax")], [N("ohg")])
        ts("dve", ohg, ohg, 1.0, 1.0e4, ALU.subtract, ALU.mult, [N("ohg")], [N("ohg")])
        em = rt[:, 36:52]
        tt("dve", em.rearrange("p (g e) -> p g e", g=4), lg[:, 4:20].rearrange("p (g e) -> p g e", g=4),
           ohg.unsqueeze(2).to_broadcast([128, 4, 4]), ALU.add, [N("lg"), N("ohg")], [N("em")])
        top8 = rt[:, 52:60]
        P.op("dve", lambda e, o=top8, i_=em: e.max(o, i_), reads=[N("em")], writes=[N("top8")])
        nv1 = rt[:, 60:61]
        ts("dve", nv1, top8[:, 0:1], -1.0, None, ALU.mult, ALU.bypass, [N("top8")], [N("nv1")])
        sel = rt[:, 64:80]
        ts("dve", sel, em, top8[:, 1:2], None, ALU.is_ge, ALU.bypass, [N("em"), N("top8")], [N("sel")])
        ex = rt[:, 80:96]
        act(ex, em, AF.Exp, [N("em"), N("nv1")], [N("ex")], bias=nv1)
        e2 = rt[:, 61:62]
        act(e2, top8[:, 1:2], AF.Exp, [N("top8"), N("nv1")], [N("e2")], bias=nv1)
        ts("dve", e2, e2, 1.0, None, ALU.add, ALU.bypass, [N("e2")], [N("e2")])
        P.op("dve", lambda e, o=e2: e.reciprocal(o, o), reads=[N("e2")], writes=[N("e2")])
        tt("dve", e2, e2, gate, ALU.mult, [N("e2"), N("gate")], [N("e2")])
        tt("dve", ex, ex, sel, ALU.mult, [N("ex"), N("sel")], [N("ex")])
        comb = rt[:, 96:112]
        ts("dve", comb, ex, e2, None, ALU.mult, ALU.bypass, [N("ex"), N("e2")], [N("comb")])
        b = bank()[0]
        mm(psv(b, 0, 128)[0:16, :], comb, ident_f, True, True, [N("comb"), "ident_f"], [R(b)])
        cp("dve", cstage[0:16, :], psv(b, 0, 128)[0:16, :], [R(b)], ["cstage"])
        P.dma("pool", "combw", comb_scr.ap()[:, i * 128:(i + 1) * 128], cstage[0:16, :], reads=["cstage"], writes=["comb_scr"])

    recload(0)
    c1_main(0)
    for i in range(NB):
        if i + 1 < NB:
            c1_main(i + 1)
        c1_router(i)

    w1_v = T["w1"].rearrange("(e k p) f -> e p k f", e=16, p=128)
    w3_v = T["w3"].rearrange("(e k p) f -> e p k f", e=16, p=128)
    w2_v = T["w2"].rearrange("(e k p) d -> e p k d", e=16, p=128)
    sidx = [0]
    ALLRA = []
    P.dma("sp", "c_g2bc", g2bc, modv[:, 5 * D:6 * D].to_broadcast([128, D]), reads=["mod_scr"], writes=["g2bc"] + ALLRA)

    def stage_cast(src_ap, dst, rname, a, g2=False, extra=()):
        s_ = sidx[0] % 2
        sidx[0] += 1
        sb = stg[s_].rearrange("p (a b) -> p a b", a=a)
        P.dma("sp", "stg%d" % s_, sb, src_ap, writes=["stg%d" % s_])
        eng_ = "pool" if s_ == 0 else "dve"
        if g2:
            tt(eng_, dst, sb, g2bc.unsqueeze(1).to_broadcast([128, a, D]), ALU.mult, ["stg%d" % s_, "g2bc"], [rname] + list(extra))
        else:
            cp(eng_, dst, sb, ["stg%d" % s_], [rname] + list(extra))

    def stage_expert(e_):
        wb = wexp[e_ % 2]
        wn = "wexp%d" % (e_ % 2)
        for kk in range(4):
            stage_cast(w1_v[e_][:, kk * 2:(kk + 1) * 2, :], wb[0][:, kk * 2:(kk + 1) * 2, :], wn + "a", 2)
        for kk in range(4):
            stage_cast(w3_v[e_][:, kk * 2:(kk + 1) * 2, :], wb[1][:, kk * 2:(kk + 1) * 2, :], wn + "b", 2)
        for kk in range(4):
            stage_cast(w2_v[e_][:, kk:kk + 1, :], wb[2][:, kk:kk + 1, :], wn + "c", 1, g2=True)

    def moe_H(e_, tg):
        wb = wexp[e_ % 2]
        wn = "wexp%d" % (e_ % 2)
        allh = ["h2T%d" % (tg * 4 + x) for x in range(4)]
        ci = (e_ * 4 + tg) % 2
        cb_ = cbt[ci]
        cn = "cbt%d" % ci
        P.dma("act", cn, cb_, comb_scr.ap()[e_:e_ + 1, tg * 512:(tg + 1) * 512].to_broadcast([128, 512]), reads=["comb_scr"], writes=[cn])
        hb_ = he[ci]
        hn = "he%d" % ci
        for ft in range(4):
            b1 = bank()[0]
            for k in range(8):
                mm(psv(b1), wb[0][:, k, ft * 128:(ft + 1) * 128], h2T[:, k, tg * 512:(tg + 1) * 512], k == 0, k == 7, [wn + "a"] + allh, [R(b1)])
            b3 = bank()[0]
            for k in range(8):
                mm(psv(b3), wb[1][:, k, ft * 128:(ft + 1) * 128], h2T[:, k, tg * 512:(tg + 1) * 512], k == 0, k == 7, [wn + "b"] + allh, [R(b3)])
            s1 = s1t[ft % 2]; t3 = t3t[ft % 2]
            act(s1, psv(b1), AF.Silu, [R(b1)], ["s1%d" % (ft % 2)])
            tt("dve", t3, psv(b3), cb_, ALU.mult, [R(b3), cn], ["t3%d" % (ft % 2)])
            tt("pool", hb_[:, ft, :], s1, t3, ALU.mult, ["s1%d" % (ft % 2), "t3%d" % (ft % 2)], [hn])

    def moe_Y(e_, tg):
        wb = wexp[e_ % 2]
        wn = "wexp%d" % (e_ % 2)
        ci = (e_ * 4 + tg) % 2
        hb_ = he[ci]
        hn = "he%d" % ci
        for tb in range(4):
            i = tg * 4 + tb
            for half in range(2):
                by = bank()[0]
                for ft in range(4):
                    mm(psv(by), hb_[:, ft, tb * 128:(tb + 1) * 128], wb[2][:, ft, half * 512:(half + 1) * 512], ft == 0, ft == 3,
                       [hn, wn + "c"], [R(by)])
                tt("dve", acc[:, i, half * 512:(half + 1) * 512], acc[:, i, half * 512:(half + 1) * 512], psv(by), ALU.add,
                   [R(by), "acc%d" % i], ["acc%d" % i])

    P.barrier()
    stage_expert(0)
    stage_expert(1)
    msteps = [(e_, tg) for e_ in range(16) for tg in range(4)]
    moe_H(*msteps[0])
    for si in range(len(msteps)):
        if si + 1 < len(msteps):
            moe_H(*msteps[si + 1])
        moe_Y(*msteps[si])
        e_, tg = msteps[si]
        if tg == 3 and e_ + 2 < 16:
            stage_expert(e_ + 2)

    out_v = out_d.rearrange("(i p) d -> p i d", p=128)
    for i in range(NB):
        P.dma("sp", "outw", out_v[:, i, :], acc[:, i, :], reads=["acc%d" % i], writes=["out"])
    P.final_wait("sp", ["out"])
    P.emit()
    return nc


_CACHE = {}


def kernel(**inputs):
    x = np.asarray(inputs["x"], np.float32)
    B = x.shape[0]
    f32 = lambda a: np.ascontiguousarray(np.asarray(a, np.float32))
    consts = _consts()
    shared = {
        "w_ada": f32(inputs["w_ada"][0]), "b_ada": f32(inputs["b_ada"][0])[None, :],
        "n1g_col": f32(np.asarray(inputs["norm1_g"][0]).reshape(8, 128).T), "n2g": f32(inputs["norm2_g"][0])[None, :],
        "w_in": f32(inputs["w_in"][0]), "lb_logits": f32(inputs["lb_logits"]),
        "rec_out_g": f32(inputs["rec_out_g"][0])[None, :], "q_norm_g": f32(inputs["q_norm_g"][0])[None, :],
        "k_norm_g": f32(inputs["k_norm_g"][0])[None, :], "ikg": f32(inputs["idx_k_norm_g"][0])[None, :],
        "ikb": f32(inputs["idx_k_norm_b"][0])[None, :], "attn_out_g": f32(inputs["attn_out_g"][0])[None, :],
        "rel_bias": f32(inputs["rel_bias"]), "w_out": f32(inputs["w_out"][0]),
        "w_r": f32(np.concatenate([np.asarray(inputs["w_rg"][0]), np.asarray(inputs["w_re"][0])], axis=1)),
        "b_r": f32(np.concatenate([np.asarray(inputs["b_rg"][0]), np.asarray(inputs["b_re"][0])]))[None, :],
        "w1": f32(np.asarray(inputs["w1"][0]).reshape(16 * D, 512)), "w3": f32(np.asarray(inputs["w3"][0]).reshape(16 * D, 512)),
        "w2": f32(np.asarray(inputs["w2"][0]).reshape(16 * 512, D)),
    }
    shared.update(consts)
    c = np.asarray(inputs["c"], np.float32)
    in_maps = []
    for core in range(8):
        b, j = core // 4, core % 4
        off = (3 - j) * 128
        xT = np.zeros((D, S), np.float32)
        xT[:, off:] = x[b, :S - off, :].T
        valid = np.ones((S,), np.float32)
        valid[:off] = 0.0
        dp = np.zeros((1, 512), np.float32)
        dp[0, :off] = -BIG
        own = np.concatenate([x[b, (4 * i + j) * 128:(4 * i + j + 1) * 128, :] for i in range(NB)], axis=0)
        m = dict(shared)
        m.update({
            "xT": xT, "x_own": f32(own), "valid": f32(valid.reshape(NPT, 128).T), "dpen": dp.astype(ml_dtypes.bfloat16),
            "c_col": f32(c[b].reshape(8, 128).T),
        })
        in_maps.append(m)
    if "nc" not in _CACHE:
        _CACHE["nc"] = build_program()
    nc = _CACHE["nc"]
    res = run_bass_kernel_spmd(nc, in_maps, core_ids=list(range(8)))
    out = np.zeros((B, S, D), np.float32)
    for core in range(8):
        b, j = core // 4, core % 4
        o = np.asarray(res.results[core]["out"], np.float32)
        for i in range(NB):
            out[b, (4 * i + j) * 128:(4 * i + j + 1) * 128, :] = o[i * 128:(i + 1) * 128, :]
    return out
```
